# Optimizing a Trainium2 kernel written in Bass

```python
import jax, jax.numpy as jnp
from jax import lax
import numpy as np

D_MODEL = 2048
BATCH = 2
SEQ = 16384
DEPTH = 1

HEAD_DIM = 128
MIX_WIDTH = D_MODEL
GDN_WIDTH = MIX_WIDTH // 2
MOBA_WIDTH = MIX_WIDTH - GDN_WIDTH
GDN_HEADS = GDN_WIDTH // HEAD_DIM
MOBA_HEADS = MOBA_WIDTH // HEAD_DIM
CONV_WIDTH = 4
GDN_CHUNK = 64
MOBA_BLOCK = 256
MOBA_TOPK = 3
MOBA_QCHUNK = 16
MEM_LEN = 256
MEM_HEADS = 4
MEM_WIDTH = MEM_HEADS * HEAD_DIM
D_FF = 4 * D_MODEL
NORM_EPS = 1e-6
IN_SPLITS = (GDN_WIDTH, GDN_WIDTH, GDN_WIDTH, GDN_WIDTH, GDN_HEADS, GDN_HEADS,
             MOBA_WIDTH, MOBA_WIDTH, MOBA_WIDTH)
IN_WIDTH = sum(IN_SPLITS)

kernel_name = 'hybrid_gdn_moba_block'

F32 = jnp.float32


def rms_norm(x, w):
    xf = x.astype(F32)
    y = xf * lax.rsqrt(jnp.mean(xf * xf, axis=-1, keepdims=True) + NORM_EPS)
    return (y * w.astype(F32)).astype(x.dtype)


def l2_norm(x):
    return x * lax.rsqrt(jnp.sum(x * x, axis=-1, keepdims=True) + NORM_EPS)


def alibi_slopes(n):
    return jnp.exp2(-8.0 * jnp.arange(1, n + 1, dtype=F32) / n)


def causal_depthwise_conv(x, w):
    c = x.shape[-1]
    return lax.conv_general_dilated(
        x, w.reshape(CONV_WIDTH, 1, c).astype(x.dtype), window_strides=(1,),
        padding=[(CONV_WIDTH - 1, 0)], dimension_numbers=('NWC', 'WIO', 'NWC'),
        feature_group_count=c)


def gated_delta_rule_chunked(q, k, v, g, beta):
    b, h, t, dk = q.shape
    dv = v.shape[-1]
    n = t // GDN_CHUNK
    q, k, v = (a.reshape(b, h, n, GDN_CHUNK, -1) for a in (q, k, v))
    g = jnp.cumsum(g.reshape(b, h, n, GDN_CHUNK), axis=-1)
    beta = beta.reshape(b, h, n, GDN_CHUNK)
    idx = jnp.arange(GDN_CHUNK)
    causal = idx[:, None] >= idx[None, :]
    strict = idx[:, None] > idx[None, :]
    decay = jnp.exp(jnp.where(causal, g[..., :, None] - g[..., None, :], -jnp.inf))
    k_beta = k * beta[..., None]
    m = jnp.where(strict, jnp.einsum('bhncd,bhnsd->bhncs', k_beta, k) * decay, 0.0)
    eye = jnp.eye(GDN_CHUNK, dtype=F32)
    t_mat = lax.linalg.triangular_solve(eye + m, jnp.broadcast_to(eye, m.shape),
                                        left_side=True, lower=True, unit_diagonal=True)
    u = jnp.einsum('bhncs,bhnsd->bhncd', t_mat, v * beta[..., None])
    w = jnp.einsum('bhncs,bhnsd->bhncd', t_mat, k_beta * jnp.exp(g)[..., None])
    a_qk = jnp.einsum('bhncd,bhnsd->bhncs', q, k) * decay
    q_g = q * jnp.exp(g)[..., None]
    k_tail = k * jnp.exp(g[..., -1:] - g)[..., None]
    g_last = jnp.exp(g[..., -1])

    def step(s, inp):
        u_n, w_n, qg_n, a_n, kt_n, gl_n = inp
        v_new = u_n - jnp.einsum('bhck,bhkv->bhcv', w_n, s)
        o = jnp.einsum('bhck,bhkv->bhcv', qg_n, s) + jnp.einsum('bhcs,bhsv->bhcv', a_n, v_new)
        s = s * gl_n[..., None, None] + jnp.einsum('bhck,bhcv->bhkv', kt_n, v_new)
        return s, o

    xs = tuple(jnp.moveaxis(a, 2, 0) for a in (u, w, q_g, a_qk, k_tail, g_last))
    s0 = jnp.zeros((b, h, dk, dv), F32)
    _, o = lax.scan(step, s0, xs)
    return jnp.moveaxis(o, 0, 2).reshape(b, h, t, dv)


def gdn_branch(q, k, v, z, a, bt, conv_w, a_log, dt_bias, norm_w):
    b, t, _ = q.shape
    qkv = jax.nn.silu(causal_depthwise_conv(jnp.concatenate([q, k, v], axis=-1), conv_w))
    q, k, v = jnp.split(qkv, 3, axis=-1)
    heads = lambda y: y.reshape(b, t, GDN_HEADS, HEAD_DIM).transpose(0, 2, 1, 3).astype(F32)
    q = l2_norm(heads(q)) * HEAD_DIM ** -0.5
    k = l2_norm(heads(k))
    v = heads(v)
    beta = jax.nn.sigmoid(bt.astype(F32)).transpose(0, 2, 1)
    g = -(jnp.exp(a_log.astype(F32)) * jax.nn.softplus(a.astype(F32) + dt_bias.astype(F32)))
    o = gated_delta_rule_chunked(q, k, v, g.transpose(0, 2, 1), beta).transpose(0, 2, 1, 3)
    gate = jax.nn.silu(z.reshape(b, t, GDN_HEADS, HEAD_DIM).astype(F32))
    o = rms_norm(o, norm_w) * gate
    return o.reshape(b, t, GDN_WIDTH).astype(z.dtype)


def moba_attention(q, k, v, slopes):
    b, h, t, d = q.shape
    tp = -(-t // MOBA_BLOCK) * MOBA_BLOCK
    pad = ((0, 0), (0, 0), (0, tp - t), (0, 0))
    q, k, v = jnp.pad(q, pad), jnp.pad(k, pad), jnp.pad(v, pad)
    nb = tp // MOBA_BLOCK
    kb = k.reshape(b, h, nb, MOBA_BLOCK, d)
    vb = v.reshape(b, h, nb, MOBA_BLOCK, d)
    kmean = jnp.mean(kb.astype(F32), axis=3)
    topk = min(MOBA_TOPK, nb)
    bi = jnp.arange(b)[:, None, None, None]
    hi = jnp.arange(h)[None, :, None, None]
    blk_ids = jnp.arange(nb)
    in_blk = jnp.arange(MOBA_BLOCK)

    def one_chunk(c):
        t0 = c * MOBA_QCHUNK
        blk = t0 // MOBA_BLOCK
        qc = lax.dynamic_slice_in_dim(q, t0, MOBA_QCHUNK, axis=2)
        tpos = t0 + jnp.arange(MOBA_QCHUNK)
        gate = jnp.einsum('bhqd,bhnd->bhqn', qc.astype(F32), kmean)
        gate = jnp.where(blk_ids < blk, gate, -jnp.inf)
        _, sel = lax.top_k(gate, topk)
        valid = jnp.arange(topk) < blk
        ks = kb[bi, hi, sel]
        vs = vb[bi, hi, sel]
        s_sel = jnp.einsum('bhqd,bhqjsd->bhqjs', qc, ks).astype(F32)
        dist_sel = (tpos[None, None, :, None, None]
                    - (sel[..., None] * MOBA_BLOCK + in_blk)).astype(F32)
        s_sel = jnp.where(valid[:, None], s_sel - slopes[None, :, None, None, None] * dist_sel,
                          -jnp.inf)
        ko = lax.dynamic_slice_in_dim(k, blk * MOBA_BLOCK, MOBA_BLOCK, axis=2)
        vo = lax.dynamic_slice_in_dim(v, blk * MOBA_BLOCK, MOBA_BLOCK, axis=2)
        dist_own = tpos[:, None] - (blk * MOBA_BLOCK + in_blk)[None, :]
        s_own = jnp.einsum('bhqd,bhsd->bhqs', qc, ko).astype(F32)
        s_own = jnp.where(dist_own >= 0,
                          s_own - slopes[None, :, None, None] * dist_own.astype(F32), -jnp.inf)
        n_sel = topk * MOBA_BLOCK
        p = jax.nn.softmax(jnp.concatenate(
            [s_sel.reshape(b, h, MOBA_QCHUNK, n_sel), s_own], axis=-1), axis=-1).astype(v.dtype)
        p_sel = p[..., :n_sel].reshape(b, h, MOBA_QCHUNK, topk, MOBA_BLOCK)
        return (jnp.einsum('bhqjs,bhqjsd->bhqd', p_sel, vs)
                + jnp.einsum('bhqs,bhsd->bhqd', p[..., n_sel:], vo))

    out = lax.map(one_chunk, jnp.arange(tp // MOBA_QCHUNK))
    return jnp.moveaxis(out, 0, 2).reshape(b, h, tp, d)[:, :, :t]


def moba_branch(q, k, v):
    b, t, _ = q.shape
    heads = lambda y: y.reshape(b, t, MOBA_HEADS, HEAD_DIM).transpose(0, 2, 1, 3)
    o = moba_attention(heads(q) * HEAD_DIM ** -0.5, heads(k), heads(v), alibi_slopes(MOBA_HEADS))
    return o.transpose(0, 2, 1, 3).reshape(b, t, MOBA_WIDTH)


def memory_cross_attention(hx, hm, w_q, w_k, w_v, w_o):
    b, t, _ = hx.shape
    m = hm.shape[1]
    q = (hx @ w_q).reshape(b, t, MEM_HEADS, HEAD_DIM)
    k = (hm @ w_k).reshape(b, m, MEM_HEADS, HEAD_DIM)
    v = (hm @ w_v).reshape(b, m, MEM_HEADS, HEAD_DIM)
    s = jnp.einsum('bthd,bmhd->bhtm', q, k).astype(F32) * HEAD_DIM ** -0.5
    p = jax.nn.softmax(s, axis=-1).astype(v.dtype)
    o = jnp.einsum('bhtm,bmhd->bthd', p, v).reshape(b, t, MEM_WIDTH)
    return o @ w_o


def setup_inputs(seed: int = 0) -> dict:
    key = jax.random.key(seed)
    ks = jax.random.split(key, 24)
    nrm = lambda kk, shape, fan_in: jax.random.normal(kk, shape, F32) * fan_in ** -0.5
    gain = lambda kk, n: 1.0 + 0.02 * jax.random.normal(kk, (DEPTH, n), F32)
    dt = jnp.exp(jax.random.uniform(ks[5], (DEPTH, GDN_HEADS), F32, np.log(1e-3), np.log(1e-1)))
    return {
        'x': jax.random.normal(ks[0], (BATCH, SEQ, D_MODEL), F32),
        'mem': jax.random.normal(ks[1], (BATCH, MEM_LEN, D_MODEL), F32),
        'pre_mix_norm': gain(ks[2], D_MODEL),
        'w_in': nrm(ks[3], (DEPTH, D_MODEL, IN_WIDTH), D_MODEL),
        'conv_w': nrm(ks[4], (DEPTH, CONV_WIDTH, 3 * GDN_WIDTH), CONV_WIDTH),
        'a_log': jnp.log(jax.random.uniform(ks[6], (DEPTH, GDN_HEADS), F32, 1.0, 16.0)),
        'dt_bias': dt + jnp.log(-jnp.expm1(-dt)),
        'gdn_norm_w': gain(ks[7], HEAD_DIM),
        'w_out': nrm(ks[8], (DEPTH, MIX_WIDTH, D_MODEL), MIX_WIDTH),
        'post_mix_norm': gain(ks[9], D_MODEL),
        'pre_mem_norm': gain(ks[10], D_MODEL),
        'mem_kv_norm': gain(ks[11], D_MODEL),
        'w_mq': nrm(ks[12], (DEPTH, D_MODEL, MEM_WIDTH), D_MODEL),
        'w_mk': nrm(ks[13], (DEPTH, D_MODEL, MEM_WIDTH), D_MODEL),
        'w_mv': nrm(ks[14], (DEPTH, D_MODEL, MEM_WIDTH), D_MODEL),
        'w_mo': nrm(ks[15], (DEPTH, MEM_WIDTH, D_MODEL), MEM_WIDTH),
        'post_mem_norm': gain(ks[16], D_MODEL),
        'pre_mlp_norm': gain(ks[17], D_MODEL),
        'w_up': nrm(ks[18], (DEPTH, D_MODEL, D_FF), D_MODEL),
        'w_down': nrm(ks[19], (DEPTH, D_FF, D_MODEL), D_FF),
        'post_mlp_norm': gain(ks[20], D_MODEL),
    }


def reference(x, mem, pre_mix_norm, w_in, conv_w, a_log, dt_bias, gdn_norm_w, w_out, post_mix_norm,
              pre_mem_norm, mem_kv_norm, w_mq, w_mk, w_mv, w_mo, post_mem_norm,
              pre_mlp_norm, w_up, w_down, post_mlp_norm):
    split_at = [int(s) for s in np.cumsum(IN_SPLITS)[:-1]]
    for l in range(DEPTH):
        h = rms_norm(x, pre_mix_norm[l])
        gq, gk, gv, gz, ga, gb, mq, mk, mv = jnp.split(h @ w_in[l], split_at, axis=-1)
        y_gdn = gdn_branch(gq, gk, gv, gz, ga, gb, conv_w[l], a_log[l], dt_bias[l], gdn_norm_w[l])
        y_moba = moba_branch(mq, mk, mv)
        mix = jnp.concatenate([y_gdn, y_moba], axis=-1) @ w_out[l]
        x = x + rms_norm(mix, post_mix_norm[l])
        ca = memory_cross_attention(rms_norm(x, pre_mem_norm[l]), rms_norm(mem, mem_kv_norm[l]),
                                    w_mq[l], w_mk[l], w_mv[l], w_mo[l])
        x = x + rms_norm(ca, post_mem_norm[l])
        f = jnp.square(jax.nn.relu(rms_norm(x, pre_mlp_norm[l]) @ w_up[l])) @ w_down[l]
        x = x + rms_norm(f, post_mlp_norm[l])
    return x
```

```python
import contextlib
import numpy as np
import ml_dtypes
import concourse.bass as bass
import concourse.mybir as mybir
from concourse.bass_utils import run_bass_kernel_spmd

F32 = mybir.dt.float32
BF16 = mybir.dt.bfloat16
ALU = mybir.AluOpType
AF = mybir.ActivationFunctionType
AX = mybir.AxisListType

D = 2048
NKC = D // 128
DFF = 8192
EPS = 1e-6
NEG = -30000.0


class Buf:
    __slots__ = ("w", "r", "excl")

    def __init__(self, excl=False):
        self.w = None
        self.r = []
        self.excl = excl


def PB():
    return Buf(excl=True)


class KB:
    def __init__(self, nc):
        self.nc = nc
        self.es = contextlib.ExitStack()
        self.E = {"pe": nc.tensor, "act": nc.scalar, "dve": nc.vector, "pool": nc.gpsimd, "sp": nc.sync}
        self.sem = {}
        self.cnt = {}
        for e in ("pe", "act", "dve", "pool"):
            self.sem[e] = self.es.enter_context(nc.semaphore("s_" + e))
            self.cnt[e] = 0
        self.seen = {e: {} for e in self.E}
        self.nbuf = 0
        self.cut = None
        self.nops = 0

    def sb(self, name, shape, dt):
        return self.es.enter_context(self.nc.sbuf_tensor(name, shape, dt))

    def ps(self, name, shape, dt):
        return self.es.enter_context(self.nc.psum_tensor(name, shape, dt))

    def dsem(self, name):
        s = self.es.enter_context(self.nc.semaphore(name))
        self.sem[name] = s
        self.cnt[name] = 0
        return name

    def _wait(self, eng, toks):
        best = {}
        for t in toks:
            if t is None:
                continue
            k, v = t
            if v > best.get(k, 0):
                best[k] = v
        for k, v in best.items():
            if self.seen[eng].get(k, 0) >= v:
                continue
            self.E[eng].wait_ge(self.sem[k], v)
            self.seen[eng][k] = v

    def _deps(self, eng, reads, writes):
        toks = []
        for b in reads:
            toks.append(b.w)
            if b.excl:
                for t in b.r:
                    if t[0] != eng:
                        toks.append(t)
        for b in writes:
            if b.w is not None and b.w[0] != eng:
                toks.append(b.w)
            for t in b.r:
                if t[0] != eng:
                    toks.append(t)
        return toks

    def op(self, eng, fn, reads=(), writes=()):
        self.nops += 1
        if self.cut is not None and self.nops > self.cut:
            return None
        self._wait(eng, self._deps(eng, reads, writes))
        ins = fn(self.E[eng])
        self.cnt[eng] += 1
        tok = (eng, self.cnt[eng])
        ins.then_inc(self.sem[eng], 1)
        for b in reads:
            b.r.append(tok)
        for b in writes:
            b.w = tok
            b.r = []
        return tok

    def dma(self, q, sem, out, in_, reads=(), writes=(), force=False):
        self.nops += 1
        if self.cut is not None and self.nops > self.cut and not force:
            return None
        self._wait(q, self._deps("dma", reads, writes))
        ins = self.E[q].dma_start(out=out, in_=in_)
        self.cnt[sem] += 16
        tok = (sem, self.cnt[sem])
        ins.then_inc(self.sem[sem], 16)
        for b in reads:
            b.r.append(tok)
        for b in writes:
            b.w = tok
            b.r = []
        return tok

    def finish(self, toks):
        self._wait("sp", toks)

    def close(self):
        self.es.close()


class Ring:
    def __init__(self, items):
        self.items = items
        self.i = 0

    def next(self):
        it = self.items[self.i % len(self.items)]
        self.i += 1
        return it


def _rep(v, rows=128):
    v = np.asarray(v, np.float32).reshape(1, -1)
    return np.ascontiguousarray(np.broadcast_to(v, (rows, v.shape[1])))


def build_b(NTOK):
    G = 512
    NG = NTOK // G
    nc = bass.Bass("TRN2", target_bir_lowering=False)
    dr = lambda n, s, dt, kind="ExternalInput": nc.dram_tensor(n, s, dt, kind=kind)
    x_d = dr("x", [NTOK, D], F32)
    y_d = dr("y", [NTOK, D], BF16)
    mem_d = dr("mem", [256, D], F32)
    w_out_d = dr("w_out", [D, D], F32)
    w_mq_d = dr("w_mq", [D, 512], F32)
    w_mk_d = dr("w_mk", [D, 512], F32)
    w_mv_d = dr("w_mv", [D, 512], F32)
    w_mo_d = dr("w_mo", [512, D], F32)
    w_up_d = dr("w_up", [D, DFF], F32)
    w_dn_d = dr("w_down", [DFF, D], F32)
    nrm_d = {n: dr(n, [128, D], F32) for n in
             ("post_mix_norm", "pre_mem_norm", "mem_kv_norm", "post_mem_norm", "pre_mlp_norm", "post_mlp_norm")}
    ident_d = dr("ident", [128, 128], BF16)
    out_d = dr("out", [NTOK, D], F32, kind="ExternalOutput")

    k = KB(nc)
    ident = k.sb("ident_sb", [128, 128], BF16)
    b_ident = Buf()
    xres = k.sb("xres", [128, 4, D], F32)
    b_x = [Buf() for _ in range(4)]
    tmp = k.sb("tmp", [128, 4, D], F32)
    b_tmp = [Buf() for _ in range(4)]
    hT = k.sb("hT", [128, NKC, G], BF16)
    b_hT = Buf()
    actT = k.sb("actT", [128, 32, G], BF16)
    b_actT = Buf()
    wsl = [k.sb(f"wsl{i}", [128, 8192], BF16) for i in range(2)]
    w_ring = Ring([(wsl[i], Buf(), k.dsem(f"d_w{i}")) for i in range(2)])
    nsl = [k.sb(f"nsl{i}", [128, D], F32) for i in range(2)]
    n_ring = Ring([(nsl[i], Buf(), k.dsem(f"d_n{i}")) for i in range(2)])
    ysl = [k.sb(f"ysl{i}", [128, D], BF16) for i in range(2)]
    y_ring = Ring([(ysl[i], Buf(), k.dsem(f"d_y{i}")) for i in range(2)])
    xn = k.sb("xn", [128, D], BF16)
    b_xn = Buf()
    junk, b_junk = xn, b_xn
    stat = k.sb("stat", [128, 16], F32)
    b_stat = Buf()
    relu_s = [k.sb(f"relu{i}", [128, G], F32) for i in range(2)]
    relu_ring = Ring([(relu_s[i], Buf()) for i in range(2)])
    kT = k.sb("kT", [128, 4, 256], BF16)
    b_kT = Buf()
    vS = k.sb("vS", [128, 2, 512], BF16)
    b_vS = Buf()
    qT = k.sb("qT", [128, 4, G], BF16)
    b_qT = Buf()
    oT = k.sb("oT", [128, 4, G], BF16)
    b_oT = Buf()
    pS = [k.sb(f"pS{i}", [128, 256], F32) for i in range(2)]
    p_ring = Ring([(pS[i], Buf()) for i in range(2)])
    pB = [k.sb(f"pB{i}", [128, 256], BF16) for i in range(2)]
    pb_ring = Ring([(pB[i], Buf()) for i in range(2)])
    pT = [k.sb(f"pT{i}", [128, 2, 128], BF16) for i in range(2)]
    pt_ring = Ring([(pT[i], Buf()) for i in range(2)])
    d_xs = [k.dsem(f"d_x{i}") for i in range(4)]
    d_c = k.dsem("d_c")
    d_o = k.dsem("d_o")
    acc = [k.ps(f"acc{i}", [128, 512], F32) for i in range(4)]
    acc_ring = Ring([(acc[i], PB()) for i in range(4)])
    tp = [k.ps(f"tp{i}", [128, 8, 128], BF16) for i in range(2)]
    tp_ring = Ring([(tp[i], PB()) for i in range(2)])
    sc = [k.ps(f"sc{i}", [128, 512], F32) for i in range(2)]
    sc_ring = Ring([(sc[i], PB()) for i in range(2)])

    k.dma("sp", d_c, ident[:], ident_d[:, :], writes=[b_ident])

    evac_i = [0]

    def evac_copy(out_ap, in_ap, reads, writes):
        evac_i[0] += 1
        if evac_i[0] % 2:
            return k.op("act", lambda e: e.activation(out=out_ap, in_=in_ap, func=AF.Copy), reads, writes)
        return k.op("dve", lambda e: e.tensor_copy(out=out_ap, in_=in_ap), reads, writes)

    wplan = []
    wplan.append((w_mk_d[:, :], NKC, 512))
    wplan.append((w_mv_d[:, :], NKC, 512))
    for _g in range(NG):
        for cg in range(4):
            wplan.append((w_out_d[:, cg * 512:(cg + 1) * 512], NKC, 512))
        wplan.append((w_mq_d[:, :], NKC, 512))
        for cg in range(4):
            wplan.append((w_mo_d[:, cg * 512:(cg + 1) * 512], 4, 512))
        for half in range(2):
            for fg in range(8):
                c0 = half * 4096 + fg * 512
                wplan.append((w_up_d[:, c0:c0 + 512], NKC, 512))
            for cg in range(8):
                wplan.append((w_dn_d[half * 4096:(half + 1) * 4096, cg * 256:(cg + 1) * 256], 32, 256))
    wstate = {"issued": 0, "used": 0, "q": []}

    def _issue_w():
        if wstate["issued"] >= len(wplan):
            return
        src_ap, nk, ncol = wplan[wstate["issued"]]
        wstate["issued"] += 1
        sl, b, sem = w_ring.next()
        view = sl[:, 0:nk * ncol].rearrange("p (k c) -> p k c", k=nk)
        k.dma("pool", sem, view, src_ap.rearrange("(k p) c -> p k c", p=128), writes=[b])
        wstate["q"].append((view, b, nk, ncol))

    def load_w(src_ap, nk, ncol):
        while wstate["issued"] < wstate["used"] + 2:
            if wstate["issued"] >= len(wplan):
                break
            _issue_w()
        view, b, pk, pc = wstate["q"].pop(0)
        assert (pk, pc) == (nk, ncol), (pk, pc, nk, ncol)
        wstate["used"] += 1
        return view, b

    def load_nrm(name):
        sl, b, sem = n_ring.next()
        k.dma("sp", sem, sl[:], nrm_d[name][:, :], writes=[b])
        return sl, b

    def rstd_of(src_ap, b_src, col, from_psum=False):
        k.op("act", lambda e: e.activation(out=junk[:], in_=src_ap, func=AF.Square,
                                           accum_out=stat[:, col:col + 1]),
             [b_src], [b_junk, b_stat])
        k.op("act", lambda e: e.activation(out=stat[:, col:col + 1], in_=stat[:, col:col + 1], func=AF.Ln,
                                           scale=1.0 / D, bias=eps_c[:, 0:1]), [b_stat, b_eps], [b_stat])
        k.op("act", lambda e: e.activation(out=stat[:, col:col + 1], in_=stat[:, col:col + 1], func=AF.Exp,
                                           scale=-0.5), [b_stat], [b_stat])

    eps_c = k.sb("eps_c", [128, 1], F32)
    b_eps = Buf()
    k.op("dve", lambda e: e.memset(eps_c[:], EPS), [], [b_eps])

    def transpose_tile(src_bf, b_src, t):
        for h in range(2):
            ps, bp = tp_ring.next()
            for j in range(8):
                kc = h * 8 + j
                k.op("pe", lambda e: e.transpose(out=ps[:, j, :], in_=src_bf[:, kc * 128:(kc + 1) * 128],
                                                 identity=ident[:]),
                     [b_src, b_ident], [bp])
            evac_copy(hT[:, h * 8:(h + 1) * 8, t * 128:(t + 1) * 128], ps[:], [bp], [b_hT])

    def norm_to_hT(t, gname_sl, b_g):
        rstd_of(xres[:, t, :], b_x[t], t)
        k.op("dve", lambda e: e.scalar_tensor_tensor(out=xn[:], in0=xres[:, t, :], scalar=stat[:, t:t + 1],
                                                     in1=gname_sl[:], op0=ALU.mult, op1=ALU.mult),
             [b_x[t], b_stat, b_g], [b_xn])
        transpose_tile(xn, b_xn, t)

    def post_norm_residual(t, g_sl, b_g):
        rstd_of(tmp[:, t, :], b_tmp[t], 8 + t)
        k.op("dve", lambda e: e.scalar_tensor_tensor(out=tmp[:, t, :], in0=tmp[:, t, :],
                                                     scalar=stat[:, 8 + t:9 + t], in1=g_sl[:],
                                                     op0=ALU.mult, op1=ALU.mult),
             [b_tmp[t], b_stat, b_g], [b_tmp[t]])
        k.op("dve", lambda e: e.tensor_tensor(out=xres[:, t, :], in0=xres[:, t, :], in1=tmp[:, t, :], op=ALU.add),
             [b_x[t], b_tmp[t]], [b_x[t]])

    def proj_tokmajor(w_src, K, lhs, b_lhs, first=True, ncol=512):
        for cg in range(D // ncol):
            wv, bw = load_w(w_src[:, cg * ncol:(cg + 1) * ncol], K, ncol)
            for t in range(4):
                ps, bp = acc_ring.next()
                for kc in range(K):
                    k.op("pe", lambda e: e.matmul(ps[:, 0:ncol], lhsT=lhs(kc, t), rhs=wv[:, kc, :],
                                                  start=(kc == 0), stop=(kc == K - 1)),
                         [b_lhs, bw], [bp])
                dst = tmp[:, t, cg * ncol:(cg + 1) * ncol]
                if first:
                    evac_copy(dst, ps[:, 0:ncol], [bp], [b_tmp[t]])
                else:
                    k.op("dve", lambda e: e.tensor_tensor(out=dst, in0=dst, in1=ps[:, 0:ncol], op=ALU.add),
                         [bp, b_tmp[t]], [b_tmp[t]])

    gkv, b_gkv = load_nrm("mem_kv_norm")
    for mt in range(2):
        k.dma("sp", d_xs[mt], xres[:, mt, :], mem_d[mt * 128:(mt + 1) * 128, :], writes=[b_x[mt]])
        rstd_of(xres[:, mt, :], b_x[mt], mt)
        k.op("dve", lambda e: e.scalar_tensor_tensor(out=xn[:], in0=xres[:, mt, :], scalar=stat[:, mt:mt + 1],
                                                     in1=gkv[:], op0=ALU.mult, op1=ALU.mult),
             [b_x[mt], b_stat, b_gkv], [b_xn])
        transpose_tile(xn, b_xn, mt)
    wv_, bw_ = load_w(w_mk_d[:, :], NKC, 512)
    for h in range(4):
        ps, bp = acc_ring.next()
        for kc in range(NKC):
            k.op("pe", lambda e: e.matmul(ps[:, 0:256], lhsT=wv_[:, kc, h * 128:(h + 1) * 128], rhs=hT[:, kc, 0:256],
                                          start=(kc == 0), stop=(kc == NKC - 1)), [b_hT, bw_], [bp])
        evac_copy(kT[:, h, :], ps[:, 0:256], [bp], [b_kT])
    wv_, bw_ = load_w(w_mv_d[:, :], NKC, 512)
    for mt in range(2):
        ps, bp = acc_ring.next()
        for kc in range(NKC):
            k.op("pe", lambda e: e.matmul(ps[:], lhsT=hT[:, kc, mt * 128:(mt + 1) * 128], rhs=wv_[:, kc, :],
                                          start=(kc == 0), stop=(kc == NKC - 1)), [b_hT, bw_], [bp])
        evac_copy(vS[:, mt, :], ps[:], [bp], [b_vS])

    out_toks = []
    for g in range(NG):
        t0 = g * G
        for t in range(4):
            k.dma("sp", d_xs[t], xres[:, t, :], x_d[t0 + t * 128:t0 + (t + 1) * 128, :], writes=[b_x[t]])
        for t in range(4):
            ys, by, sem = y_ring.next()
            k.dma("sp", sem, ys[:], y_d[t0 + t * 128:t0 + (t + 1) * 128, :], writes=[by])
            transpose_tile(ys, by, t)
        proj_tokmajor(w_out_d, NKC, lambda kc, t: hT[:, kc, t * 128:(t + 1) * 128], b_hT)
        gs, bg = load_nrm("post_mix_norm")
        for t in range(4):
            post_norm_residual(t, gs, bg)
        gs, bg = load_nrm("pre_mem_norm")
        for t in range(4):
            norm_to_hT(t, gs, bg)
        wq, bwq = load_w(w_mq_d[:, :], NKC, 512)
        for h in range(4):
            ps, bp = acc_ring.next()
            for kc in range(NKC):
                k.op("pe", lambda e: e.matmul(ps[:], lhsT=wq[:, kc, h * 128:(h + 1) * 128], rhs=hT[:, kc, :],
                                              start=(kc == 0), stop=(kc == NKC - 1)), [b_hT, bwq], [bp])
            k.op("act", lambda e: e.activation(out=qT[:, h, :], in_=ps[:], func=AF.Copy, scale=128.0 ** -0.5),
                 [bp], [b_qT])
        for t in range(4):
            for h in range(4):
                s_ps, bs = sc_ring.next()
                k.op("pe", lambda e: e.matmul(s_ps[:, 0:256], lhsT=qT[:, h, t * 128:(t + 1) * 128], rhs=kT[:, h, :],
                                              start=True, stop=True), [b_qT, b_kT], [bs])
                c0 = 12 + (h % 2) * 2
                k.op("dve", lambda e: e.tensor_reduce(out=stat[:, c0:c0 + 1], in_=s_ps[:, 0:256], axis=AX.X,
                                                      op=ALU.max, negate=True), [bs], [b_stat])
                p_s, bps = p_ring.next()
                k.op("act", lambda e: e.activation(out=p_s[:], in_=s_ps[:, 0:256], func=AF.Exp,
                                                   bias=stat[:, c0:c0 + 1], accum_out=stat[:, c0 + 1:c0 + 2]),
                     [bs, b_stat], [bps, b_stat])
                k.op("dve", lambda e: e.reciprocal(out=stat[:, c0 + 1:c0 + 2], in_=stat[:, c0 + 1:c0 + 2]),
                     [b_stat], [b_stat])
                p_b, bpb = pb_ring.next()
                k.op("dve", lambda e: e.tensor_scalar(out=p_b[:], in0=p_s[:], scalar1=stat[:, c0 + 1:c0 + 2],
                                                      scalar2=None, op0=ALU.mult), [bps, b_stat], [bpb])
                tps, btp = tp_ring.next()
                for mt in range(2):
                    k.op("pe", lambda e: e.transpose(out=tps[:, mt, :], in_=p_b[:, mt * 128:(mt + 1) * 128],
                                                     identity=ident[:]), [bpb, b_ident], [btp])
                p_t, bpt = pt_ring.next()
                evac_copy(p_t[:], tps[:, 0:2, :], [btp], [bpt])
                o_ps, bo = sc_ring.next()
                for mt in range(2):
                    k.op("pe", lambda e: e.matmul(o_ps[:, 0:128], lhsT=vS[:, mt, h * 128:(h + 1) * 128],
                                                  rhs=p_t[:, mt, :], start=(mt == 0), stop=(mt == 1)),
                         [b_vS, bpt], [bo])
                evac_copy(oT[:, h, t * 128:(t + 1) * 128], o_ps[:, 0:128], [bo], [b_oT])
        proj_tokmajor(w_mo_d, 4, lambda kc, t: oT[:, kc, t * 128:(t + 1) * 128], b_oT)
        gs, bg = load_nrm("post_mem_norm")
        for t in range(4):
            post_norm_residual(t, gs, bg)
        gs, bg = load_nrm("pre_mlp_norm")
        for t in range(4):
            norm_to_hT(t, gs, bg)
        for half in range(2):
            for fg in range(8):
                c0 = half * 4096 + fg * 512
                wu, bwu = load_w(w_up_d[:, c0:c0 + 512], NKC, 512)
                for j in range(4):
                    ps, bp = acc_ring.next()
                    for kc in range(NKC):
                        k.op("pe", lambda e: e.matmul(ps[:], lhsT=wu[:, kc, j * 128:(j + 1) * 128], rhs=hT[:, kc, :],
                                                      start=(kc == 0), stop=(kc == NKC - 1)), [b_hT, bwu], [bp])
                    rs, br = relu_ring.next()
                    k.op("act", lambda e: e.activation(out=rs[:], in_=ps[:], func=AF.Relu), [bp], [br])
                    fi = fg * 4 + j
                    k.op("dve", lambda e: e.tensor_tensor(out=actT[:, fi, :], in0=rs[:], in1=rs[:], op=ALU.mult),
                         [br], [b_actT])
            r0 = half * 4096
            proj_tokmajor(w_dn_d[r0:r0 + 4096, :], 32, lambda kc, t: actT[:, kc, t * 128:(t + 1) * 128], b_actT,
                          first=(half == 0), ncol=256)
        gs, bg = load_nrm("post_mlp_norm")
        for t in range(4):
            post_norm_residual(t, gs, bg)
            out_toks.append(k.dma("sp", d_o, out_d[t0 + t * 128:t0 + (t + 1) * 128, :], xres[:, t, :],
                                  reads=[b_x[t]]))
    k.finish(out_toks)
    k.close()
    return nc


def consts_a(T, core):
    NB = T // 256
    c = {}
    c["identF"] = np.eye(128, dtype=np.float32)
    c["identB"] = np.eye(128, dtype=ml_dtypes.bfloat16)
    r = np.arange(128)
    same = (r[:, None] // 64) == (r[None, :] // 64)
    c["U"] = (same & (r[:, None] <= r[None, :])).astype(np.float32)
    c["Ls"] = (same & (r[:, None] > r[None, :])).astype(np.float32)
    c["blk"] = same.astype(np.float32)
    c["onesF"] = np.ones((128, 128), np.float32)
    c["halfm"] = np.zeros((128, 16), np.float32)
    c["halfm"][:, 0] = r < 64
    c["halfm"][:, 1] = r >= 64
    c["jrow"] = _rep(np.arange(64, dtype=np.float32))
    sel = np.zeros((128, 32, 128), np.float32)
    for j in range(32):
        sel[j, j, :] = 1.0
        sel[32 + j, j, :] = 1.0
    c["sel32"] = sel.reshape(128, 32 * 128).astype(ml_dtypes.bfloat16)
    cm = np.zeros((128, 4, 512), np.float32)
    qi = np.arange(512)
    for ktl in range(4):
        key = ktl * 128 + r
        cm[:, ktl, :] = np.where(key[:, None] <= qi[None, :], 0.0, NEG)
    c["cmask"] = cm.reshape(128, 2048).astype(ml_dtypes.bfloat16)
    slope = float(2.0 ** (-8.0 * (core + 1) / 8.0))
    ntile = T // 128
    i = np.arange(ntile)
    c["alibiT"] = (slope * (r[:, None] + 128.0 * (3 - i[None, :]))).astype(np.float32)
    c["slopeT"] = (slope * (r[:, None] + 128.0 * np.arange(16)[None, :])).astype(np.float32)
    return c


CONST_A = {"identF": ([128, 128], F32), "identB": ([128, 128], BF16), "U": ([128, 128], F32),
           "Ls": ([128, 128], F32), "blk": ([128, 128], F32), "onesF": ([128, 128], F32),
           "halfm": ([128, 16], F32), "sel32": ([128, 4096], BF16), "cmask": ([128, 2048], BF16),
           "slopeT": ([128, 16], F32), "convw": ([128, 16], F32), "alog": ([128, 16], F32),
           "dtb": ([128, 16], F32), "gnw": ([128, 128], F32), "gpre": ([128, NKC], F32)}


def build_a(T, stage=3, cut=None):
    G = 512
    NG = T // G
    NB = T // 256
    NT = T // 128
    KBK = min(32, NB)
    N = 2 * T
    NW = 898
    nc = bass.Bass("TRN2", target_bir_lowering=False)
    dr = lambda n, s, dt, kind="ExternalInput": nc.dram_tensor(n, s, dt, kind=kind)
    x_d = dr("x", [N, D], F32)
    w_d = dr("w_in", [D, NW], F32)
    cd = {n: dr(n, sh, dt) for n, (sh, dt) in CONST_A.items()}
    cd["jrow"] = dr("jrow", [128, 64], F32)
    cd["alibiT"] = dr("alibiT", [128, NT], F32)
    y_d = dr("y", [N, 256], BF16, kind="ExternalOutput")

    k = KB(nc)
    k.cut = cut
    C = {}
    bC = Buf()
    d_c = k.dsem("d_c")
    for n in cd:
        sh = list(cd[n].shape)
        C[n] = k.sb("c_" + n, sh, cd[n].dtype)
        k.dma("sp", d_c, C[n][:], cd[n][:, :], writes=[bC])
    k.finish([bC.w])
    for e in ("pe", "act", "dve"):
        k._wait(e, [bC.w])
    bC = Buf()

    w_sb = k.sb("w_sb", [128, NKC, 960], BF16)
    b_w = Buf()
    wtmp = [k.sb(f"wtmp{i}", [128, NW], F32) for i in range(2)]
    wt_ring = Ring([(wtmp[i], Buf(), k.dsem(f"d_wt{i}")) for i in range(2)])
    KT_all = k.sb("KT_all", [128, T], BF16)
    b_KT = Buf()
    V_all = k.sb("V_all", [128, NT, 136], BF16)
    b_V = Buf()
    kmT = k.sb("kmT", [128, NB], F32)
    b_km = Buf()
    xt = [k.sb(f"xt{i}", [128, D], F32) for i in range(2)]
    x_ring = Ring([(xt[i], Buf(), k.dsem(f"d_xa{i}")) for i in range(2)])
    xn = k.sb("xn_a", [128, D], BF16)
    b_xn = Buf()
    hT = k.sb("hT_a", [128, NKC, G], BF16)
    b_hT = Buf()
    st = k.sb("st_a", [128, 32], F32)
    b_st = Buf()
    cst = k.sb("cst_a", [128, 8], F32)
    b_cst = Buf()
    pre = k.sb("pre", [128, 3, 515], F32)
    b_pre = Buf()
    cs = k.sb("cs", [128, 3, G], F32)
    b_cs = Buf()
    cvt = k.sb("cvt", [128, G], F32)
    b_cvt = Buf()
    rn = k.sb("rn", [128, G], F32)
    b_rn = Buf()
    QnT = k.sb("QnT", [128, G], F32)
    b_Qn = Buf()
    KnT = k.sb("KnT", [128, G], F32)
    b_Kn = Buf()
    zs = k.sb("zs", [128, 4, 128], F32)
    b_zs = [Buf() for _ in range(4)]
    ab = k.sb("ab", [128, 4, 2], F32)
    b_ab = [Buf() for _ in range(4)]
    Sst = [k.sb(f"S{i}", [128, 128], F32) for i in range(2)]
    b_S = [Buf(), Buf()]
    ybuf = [k.sb(f"ybuf{i}", [128, 256], BF16) for i in range(4)]
    b_y = [Buf() for _ in range(4)]
    d_y = [k.dsem(f"d_yo{i}") for i in range(4)]
    QT = k.sb("QT", [128, G], BF16)
    b_QT = Buf()
    QTf = k.sb("QTf", [128, G], F32)
    b_QTf = Buf()
    sqQ = k.sb("sqQ", [128, G], F32)
    b_sqQ = Buf()
    biasT = k.sb("biasT", [64, G], BF16)
    b_bT = Buf()
    PTs = [k.sb(f"PT{i}", [128, G], BF16) for i in range(3)]
    pt_ring = Ring([(PTs[i], Buf()) for i in range(3)])
    sm = [k.sb(f"sm{i}", [128, 128], F32) for i in range(10)]
    sm_ring = Ring([(sm[i], Buf()) for i in range(10)])
    named = {}

    def nb(name, par):
        key = (name, par)
        if key not in named:
            named[key] = (k.sb(f"n_{name}{par}", [128, 128], F32), Buf())
        return named[key]

    acc = [k.ps(f"a_acc{i}", [128, 512], F32) for i in range(3)]
    acc_ring = Ring([(acc[i], PB()) for i in range(3)])
    tpx = k.ps("a_tp", [128, 8, 128], BF16)
    b_tpx = PB()
    gp = [k.ps(f"a_gp{i}", [128, 4, 128], F32) for i in range(2)]
    _gpb = [PB(), PB()]
    gp_ring = Ring([(gp[i % 2][:, i // 2, :], _gpb[i % 2]) for i in range(8)])
    ops_ = [k.ps(f"a_o{i}", [128, 2, 129], F32) for i in range(2)]
    _ob = [PB(), PB()]
    b_ops = [_ob[0], _ob[0], _ob[1], _ob[1]]

    def o_ps(qt):
        return ops_[qt // 2][:, qt % 2, :]

    ev = [0]

    def evac(out_ap, in_ap, reads, writes):
        ev[0] += 1
        if ev[0] % 2:
            return k.op("act", lambda e: e.activation(out=out_ap, in_=in_ap, func=AF.Copy), reads, writes)
        return k.op("dve", lambda e: e.tensor_copy(out=out_ap, in_=in_ap), reads, writes)

    def dve(fn, reads, writes):
        return k.op("dve", fn, reads, writes)

    def act(fn, reads, writes):
        return k.op("act", fn, reads, writes)

    def pe(fn, reads, writes):
        return k.op("pe", fn, reads, writes)

    dve(lambda e: e.memset(cst[:, 0:1], EPS), [], [b_cst])
    dve(lambda e: e.memset(cst[:, 1:2], 1.0), [], [b_cst])
    act(lambda e: e.activation(out=cst[:, 2:3], in_=C["alog"][:, 0:1], func=AF.Exp), [b_cst], [b_cst])
    dve(lambda e: e.tensor_scalar(out=cst[:, 2:3], in0=cst[:, 2:3], scalar1=-1.0, scalar2=None, op0=ALU.mult),
        [b_cst], [b_cst])
    dve(lambda e: e.memset(cst[:, 4:5], 128.0 * EPS), [], [b_cst])
    dve(lambda e: e.memset(V_all[:], 1.0), [], [b_V])
    for kc in range(NKC):
        wt, bwt, sem = wt_ring.next()
        k.dma("sp", sem, wt[:], w_d[kc * 128:(kc + 1) * 128, :], writes=[bwt])
        dve(lambda e: e.tensor_scalar(out=w_sb[:, kc, 0:NW], in0=wt[:], scalar1=C["gpre"][:, kc:kc + 1], scalar2=None,
                                      op0=ALU.mult), [bwt], [b_w])

    def rstd_ln_exp(col_ap, scale, bias_col):
        act(lambda e: e.activation(out=col_ap, in_=col_ap, func=AF.Ln, scale=scale, bias=bias_col),
            [b_st, b_cst], [b_st])
        act(lambda e: e.activation(out=col_ap, in_=col_ap, func=AF.Exp, scale=-0.5), [b_st], [b_st])

    out_toks = []
    for b in range(2):
        dve(lambda e: e.memset(pre[:], 0.0), [], [b_pre])
        dve(lambda e: e.memset(Sst[0][:], 0.0), [], [b_S[0]])
        dve(lambda e: e.memset(cst[:, 3:4], 0.0), [], [b_cst])
        dve(lambda e: e.memset(kmT[:], 0.0), [], [b_km])
        scur = 0
        for g in range(NG):
            t0 = g * G
            n0 = b * T + t0
            gt0 = t0 // 128
            b0 = t0 // 256
            for t in (range(4) if stage >= 1 else []):
                xs, bx, sem = x_ring.next()
                k.dma("sp", sem, xs[:], x_d[n0 + t * 128:n0 + (t + 1) * 128, :], writes=[bx])
                act(lambda e: e.activation(out=xn[:], in_=xs[:], func=AF.Square, accum_out=st[:, t:t + 1]),
                    [bx], [b_xn, b_st])
                rstd_ln_exp(st[:, t:t + 1], 1.0 / D, cst[:, 0:1])
                dve(lambda e: e.tensor_scalar(out=xn[:], in0=xs[:], scalar1=st[:, t:t + 1], scalar2=None,
                                              op0=ALU.mult), [bx, b_st], [b_xn])
                for h in range(2):
                    for j in range(8):
                        kc = h * 8 + j
                        pe(lambda e: e.transpose(out=tpx[:, j, :], in_=xn[:, kc * 128:(kc + 1) * 128],
                                                 identity=C["identB"][:]), [b_xn], [b_tpx])
                    evac(hT[:, h * 8:(h + 1) * 8, t * 128:(t + 1) * 128], tpx[:], [b_tpx], [b_hT])
            for ci in (range(5) if stage >= 1 else []):
                ps, bp = acc_ring.next()
                for kc in range(NKC):
                    pe(lambda e: e.matmul(ps[:], lhsT=w_sb[:, kc, ci * 128:(ci + 1) * 128], rhs=hT[:, kc, :],
                                          start=(kc == 0), stop=(kc == NKC - 1)), [b_w, b_hT], [bp])
                if ci < 3:
                    evac(pre[:, ci, 3:515], ps[:], [bp], [b_pre])
                elif ci == 3:
                    act(lambda e: e.activation(out=QT[:], in_=ps[:], func=AF.Copy, scale=128.0 ** -0.5),
                        [bp], [b_QT])
                    dve(lambda e: e.tensor_scalar(out=QTf[:], in0=ps[:], scalar1=128.0 ** -0.5, scalar2=None,
                                                  op0=ALU.mult), [bp], [b_QTf])
                else:
                    act(lambda e: e.activation(out=KT_all[:, t0:t0 + G], in_=ps[:], func=AF.Copy), [bp], [b_KT])
                    dve(lambda e: e.tensor_reduce(out=kmT[:, b0:b0 + 2],
                                                  in_=ps[:].rearrange("p (a c) -> p a c", a=2),
                                                  axis=AX.X, op=ALU.add), [bp], [b_km])
                    act(lambda e: e.activation(out=cvt[:], in_=ps[:], func=AF.Square), [bp], [b_cvt])
                    p2, bp2 = acc_ring.next()
                    pe(lambda e: e.matmul(p2[:], lhsT=C["onesF"][:], rhs=cvt[:], start=True, stop=True),
                       [b_cvt], [bp2])
                    dve(lambda e: e.tensor_reduce(out=st[:, 8:9], in_=p2[:], axis=AX.X, op=ALU.max), [bp2], [b_st])
                    dve(lambda e: e.tensor_tensor(out=cst[:, 3:4], in0=cst[:, 3:4], in1=st[:, 8:9], op=ALU.max),
                        [b_st, b_cst], [b_cst])
            for t in (range(4) if stage >= 1 else []):
                ps, bp = acc_ring.next()
                for kc in range(NKC):
                    pe(lambda e: e.matmul(ps[:, 0:258], lhsT=hT[:, kc, t * 128:(t + 1) * 128], rhs=w_sb[:, kc, 640:898],
                                          start=(kc == 0), stop=(kc == NKC - 1)), [b_w, b_hT], [bp])
                act(lambda e: e.activation(out=zs[:, t, :], in_=ps[:, 0:128], func=AF.Silu), [bp], [b_zs[t]])
                dve(lambda e: e.tensor_copy(out=V_all[:, gt0 + t, 0:128], in_=ps[:, 128:256]), [bp], [b_V])
                dve(lambda e: e.tensor_copy(out=ab[:, t, :], in_=ps[:, 256:258]), [bp], [b_ab[t]])

            if stage < 3:
                for t in range(4):
                    dve(lambda e: e.memset(ybuf[t][:], 0.0), [], [b_y[t]])
            for ci in (range(3) if stage >= 2 else []):
                cw = lambda j: C["convw"][:, ci * 4 + j:ci * 4 + j + 1]
                dve(lambda e: e.tensor_scalar(out=cvt[:], in0=pre[:, ci, 0:512], scalar1=cw(0), scalar2=None,
                                              op0=ALU.mult), [b_pre], [b_cvt])
                for j in range(1, 4):
                    dve(lambda e: e.scalar_tensor_tensor(out=cvt[:], in0=pre[:, ci, j:j + 512], scalar=cw(j),
                                                         in1=cvt[:], op0=ALU.mult, op1=ALU.add),
                        [b_pre, b_cvt], [b_cvt])
                act(lambda e: e.activation(out=cs[:, ci, :], in_=cvt[:], func=AF.Silu), [b_cvt], [b_cs])
            dve(lambda e: e.tensor_copy(out=pre[:, :, 0:3], in_=pre[:, :, 512:515]), [b_pre], [b_pre])
            for ci in (range(2) if stage >= 2 else []):
                act(lambda e: e.activation(out=cvt[:], in_=cs[:, ci, :], func=AF.Square), [b_cs], [b_cvt])
                ps, bp = acc_ring.next()
                pe(lambda e: e.matmul(ps[:], lhsT=C["onesF"][:], rhs=cvt[:], start=True, stop=True), [b_cvt], [bp])
                if ci == 0:
                    act(lambda e: e.activation(out=rn[:], in_=ps[:], func=AF.Ln, scale=128.0, bias=cst[:, 4:5]),
                        [bp, b_cst], [b_rn])
                else:
                    act(lambda e: e.activation(out=rn[:], in_=ps[:], func=AF.Ln, scale=1.0, bias=cst[:, 0:1]),
                        [bp, b_cst], [b_rn])
                act(lambda e: e.activation(out=rn[:], in_=rn[:], func=AF.Exp, scale=-0.5), [b_rn], [b_rn])
                dst, bd = (QnT, b_Qn) if ci == 0 else (KnT, b_Kn)
                dve(lambda e: e.tensor_tensor(out=dst[:], in0=cs[:, ci, :], in1=rn[:], op=ALU.mult),
                    [b_cs, b_rn], [bd])

            for t in (range(4) if stage >= 2 else []):
                par = t % 2
                tsl = slice(t * 128, (t + 1) * 128)
                c0 = 12 + 0
                act(lambda e: e.activation(out=st[:, 12:13], in_=ab[:, t, 0:1], func=AF.Exp, bias=C["dtb"][:, 0:1]),
                    [b_ab[t]], [b_st])
                act(lambda e: e.activation(out=st[:, 12:13], in_=st[:, 12:13], func=AF.Ln, bias=cst[:, 1:2]),
                    [b_st, b_cst], [b_st])
                dve(lambda e: e.tensor_scalar(out=st[:, 12:13], in0=st[:, 12:13], scalar1=cst[:, 2:3], scalar2=None,
                                              op0=ALU.mult), [b_st, b_cst], [b_st])
                act(lambda e: e.activation(out=st[:, 13:14], in_=ab[:, t, 1:2], func=AF.Sigmoid), [b_ab[t]], [b_st])
                dve(lambda e: e.tensor_scalar(out=st[:, 14:16], in0=C["halfm"][:, 0:2], scalar1=st[:, 12:13], scalar2=None,
                                              op0=ALU.mult), [b_st], [b_st])
                gps, bgp = gp_ring.next()
                pe(lambda e: e.matmul(gps[:, 0:1], lhsT=C["U"][:], rhs=st[:, 12:13], start=True, stop=True),
                   [b_st], [bgp])
                pe(lambda e: e.matmul(gps[:, 1:2], lhsT=C["blk"][:], rhs=st[:, 12:13], start=True, stop=True),
                   [b_st], [bgp])
                pe(lambda e: e.matmul(gps[:, 2:4], lhsT=C["onesF"][:], rhs=st[:, 14:16], start=True, stop=True),
                   [b_st], [bgp])
                dve(lambda e: e.tensor_copy(out=st[:, 16:20], in_=gps[:, 0:4]), [bgp], [b_st])
                dve(lambda e: e.tensor_tensor(out=st[:, 20:21], in0=st[:, 17:18], in1=st[:, 16:17], op=ALU.subtract),
                    [b_st], [b_st])
                act(lambda e: e.activation(out=st[:, 21:22], in_=st[:, 16:17], func=AF.Exp), [b_st], [b_st])
                act(lambda e: e.activation(out=st[:, 22:23], in_=st[:, 20:21], func=AF.Exp), [b_st], [b_st])
                act(lambda e: e.activation(out=st[:, 23:25], in_=st[:, 18:20], func=AF.Exp), [b_st], [b_st])
                dve(lambda e: e.tensor_tensor(out=st[:, 25:26], in0=st[:, 13:14], in1=st[:, 21:22], op=ALU.mult),
                    [b_st], [b_st])
                g_c, beta_c, egc_c, ekt_c, bg_c = (st[:, 12:13], st[:, 13:14], st[:, 21:22], st[:, 22:23],
                                                   st[:, 25:26])
                egl = nb("egl", par)
                dve(lambda e: e.tensor_copy(out=egl[0][:, 0:2], in_=st[:, 23:25]), [b_st], [egl[1]])
                Kbg, Ktl, Vb = nb("Kbg", par), nb("Ktl", par), nb("Vb", par)
                gps, bgp = gp_ring.next()
                pe(lambda e: e.transpose(out=gps[:], in_=KnT[:, tsl], identity=C["identF"][:]), [b_Kn], [bgp])
                dve(lambda e: e.tensor_scalar(out=Kbg[0][:], in0=gps[:], scalar1=bg_c, scalar2=None, op0=ALU.mult),
                    [bgp, b_st], [Kbg[1]])
                dve(lambda e: e.tensor_scalar(out=Ktl[0][:], in0=gps[:], scalar1=ekt_c, scalar2=None, op0=ALU.mult),
                    [bgp, b_st], [Ktl[1]])
                gps, bgp = gp_ring.next()
                pe(lambda e: e.transpose(out=gps[:], in_=cs[:, 2, tsl], identity=C["identF"][:]), [b_cs], [bgp])
                dve(lambda e: e.tensor_scalar(out=Vb[0][:], in0=gps[:], scalar1=beta_c, scalar2=None, op0=ALU.mult),
                    [bgp, b_st], [Vb[1]])
                Am, bAm = sm_ring.next()
                dve(lambda e: e.tensor_scalar(out=Am[:], in0=C["U"][:], scalar1=g_c, scalar2=None, op0=ALU.mult),
                    [b_st], [bAm])
                EL, bEL = sm_ring.next()
                EU, bEU = sm_ring.next()
                gps, bgp = gp_ring.next()
                pe(lambda e: e.matmul(gps[:], lhsT=Am[:], rhs=C["Ls"][:], start=True, stop=True), [bAm], [bgp])
                act(lambda e: e.activation(out=EL[:], in_=gps[:], func=AF.Exp), [bgp], [bEL])
                dve(lambda e: e.tensor_tensor(out=EL[:], in0=EL[:], in1=C["Ls"][:], op=ALU.mult), [bEL], [bEL])
                gps, bgp = gp_ring.next()
                pe(lambda e: e.matmul(gps[:], lhsT=C["Ls"][:], rhs=Am[:], start=True, stop=True), [bAm], [bgp])
                act(lambda e: e.activation(out=EU[:], in_=gps[:], func=AF.Exp), [bgp], [bEU])
                dve(lambda e: e.tensor_tensor(out=EU[:], in0=EU[:], in1=C["U"][:], op=ALU.mult), [bEU], [bEU])
                Mm, bM = sm_ring.next()
                gps, bgp = gp_ring.next()
                pe(lambda e: e.matmul(gps[:], lhsT=KnT[:, tsl], rhs=KnT[:, tsl], start=True, stop=True), [b_Kn], [bgp])
                dve(lambda e: e.scalar_tensor_tensor(out=Mm[:], in0=gps[:], scalar=beta_c, in1=EL[:],
                                                     op0=ALU.mult, op1=ALU.mult), [bgp, b_st, bEL], [bM])
                Aq = nb("Aq", par)
                gps, bgp = gp_ring.next()
                pe(lambda e: e.matmul(gps[:], lhsT=KnT[:, tsl], rhs=QnT[:, tsl], start=True, stop=True),
                   [b_Kn, b_Qn], [bgp])
                dve(lambda e: e.tensor_tensor(out=Aq[0][:], in0=gps[:], in1=EU[:], op=ALU.mult), [bgp, bEU], [Aq[1]])
                Nm, bN = sm_ring.next()
                gps, bgp = gp_ring.next()
                pe(lambda e: e.transpose(out=gps[:], in_=Mm[:], identity=C["identF"][:]), [bM], [bgp])
                evac(Nm[:], gps[:], [bgp], [bN])
                Pm, bPm = sm_ring.next()
                Pn, bPn = sm_ring.next()
                dve(lambda e: e.tensor_tensor(out=Pm[:], in0=C["identF"][:], in1=Mm[:], op=ALU.subtract), [bM], [bPm])
                dve(lambda e: e.tensor_tensor(out=Pn[:], in0=C["identF"][:], in1=Nm[:], op=ALU.subtract), [bN], [bPn])
                for lvl in range(5):
                    last = lvl == 4
                    N2, bN2 = sm_ring.next()
                    gps, bgp = gp_ring.next()
                    pe(lambda e: e.matmul(gps[:], lhsT=Mm[:], rhs=Nm[:], start=True, stop=True), [bM, bN], [bgp])
                    evac(N2[:], gps[:], [bgp], [bN2])
                    if not last:
                        M2, bM2 = sm_ring.next()
                        gps, bgp = gp_ring.next()
                        pe(lambda e: e.matmul(gps[:], lhsT=Nm[:], rhs=Mm[:], start=True, stop=True), [bM, bN], [bgp])
                        evac(M2[:], gps[:], [bgp], [bM2])
                    Pn2, bPn2 = sm_ring.next()
                    gps, bgp = gp_ring.next()
                    pe(lambda e: e.matmul(gps[:], lhsT=Pm[:], rhs=N2[:], start=True, stop=True), [bPm, bN2], [bgp])
                    dve(lambda e: e.tensor_tensor(out=Pn2[:], in0=gps[:], in1=Pn[:], op=ALU.add), [bgp, bPn], [bPn2])
                    if not last:
                        Pm2, bPm2 = sm_ring.next()
                        gps, bgp = gp_ring.next()
                        pe(lambda e: e.matmul(gps[:], lhsT=Pn[:], rhs=M2[:], start=True, stop=True), [bPn, bM2], [bgp])
                        dve(lambda e: e.tensor_tensor(out=Pm2[:], in0=gps[:], in1=Pm[:], op=ALU.add),
                            [bgp, bPm], [bPm2])
                        Pm, bPm, Mm, bM = Pm2, bPm2, M2, bM2
                    Pn, bPn, Nm, bN = Pn2, bPn2, N2, bN2
                TT, bTT = Pn, bPn
                u_, wT_, qg_ = nb("u", par), nb("wT", par), nb("qg", par)
                gps, bgp = gp_ring.next()
                pe(lambda e: e.matmul(gps[:], lhsT=TT[:], rhs=Vb[0][:], start=True, stop=True), [bTT, Vb[1]], [bgp])
                evac(u_[0][:], gps[:], [bgp], [u_[1]])
                gps, bgp = gp_ring.next()
                pe(lambda e: e.matmul(gps[:], lhsT=Kbg[0][:], rhs=TT[:], start=True, stop=True), [bTT, Kbg[1]], [bgp])
                evac(wT_[0][:], gps[:], [bgp], [wT_[1]])
                dg, bdg = sm_ring.next()
                dve(lambda e: e.tensor_scalar(out=dg[:], in0=C["identF"][:], scalar1=egc_c, scalar2=None,
                                              op0=ALU.mult), [b_st], [bdg])
                gps, bgp = gp_ring.next()
                pe(lambda e: e.matmul(gps[:], lhsT=C["onesF"][:], rhs=dg[:], start=True, stop=True), [bdg], [bgp])
                dve(lambda e: e.tensor_tensor(out=qg_[0][:], in0=gps[:], in1=QnT[:, tsl], op=ALU.mult),
                    [bgp, b_Qn], [qg_[1]])
                o_sb = nb("o", par)
                vn = nb("vn", par)
                for j in range(2):
                    rs = slice(64 * j, 64 * j + 64)
                    S_, bS_ = Sst[scur], b_S[scur]
                    Sn, bSn = Sst[1 - scur], b_S[1 - scur]
                    gps, bgp = gp_ring.next()
                    pe(lambda e: e.matmul(gps[:], lhsT=wT_[0][:], rhs=S_[:], start=True, stop=True),
                       [wT_[1], bS_], [bgp])
                    dve(lambda e: e.tensor_tensor(out=vn[0][rs, :], in0=u_[0][rs, :], in1=gps[rs, :], op=ALU.subtract),
                        [bgp, u_[1]], [vn[1]])
                    gpo, bgo = gp_ring.next()
                    pe(lambda e: e.matmul(gpo[:], lhsT=qg_[0][:], rhs=S_[:], start=True, stop=False),
                       [qg_[1], bS_], [bgo])
                    pe(lambda e: e.matmul(gpo[:], lhsT=Aq[0][rs, :], rhs=vn[0][rs, :], start=False, stop=True),
                       [Aq[1], vn[1]], [bgo])
                    gps, bgp = gp_ring.next()
                    pe(lambda e: e.matmul(gps[:], lhsT=Ktl[0][rs, :], rhs=vn[0][rs, :], start=True, stop=True),
                       [Ktl[1], vn[1]], [bgp])
                    dve(lambda e: e.scalar_tensor_tensor(out=Sn[:], in0=S_[:], scalar=egl[0][:, j:j + 1], in1=gps[:],
                                                         op0=ALU.mult, op1=ALU.add), [bS_, egl[1], bgp], [bSn])
                    act(lambda e: e.activation(out=o_sb[0][rs, :], in_=gpo[rs, :], func=AF.Copy), [bgo], [o_sb[1]])
                    scur = 1 - scur
                jk, bjk = sm_ring.next()
                act(lambda e: e.activation(out=jk[:], in_=o_sb[0][:], func=AF.Square, accum_out=st[:, 26:27]),
                    [o_sb[1]], [bjk, b_st])
                rstd_ln_exp(st[:, 26:27], 1.0 / 128, cst[:, 0:1])
                dve(lambda e: e.scalar_tensor_tensor(out=jk[:], in0=o_sb[0][:], scalar=st[:, 26:27], in1=C["gnw"][:],
                                                     op0=ALU.mult, op1=ALU.mult), [o_sb[1], b_st, bjk], [bjk])
                dve(lambda e: e.tensor_tensor(out=ybuf[t][:, 0:128], in0=jk[:], in1=zs[:, t, :], op=ALU.mult),
                    [bjk, b_zs[t]], [b_y[t]])

            act(lambda e: e.activation(out=sqQ[:], in_=QTf[:], func=AF.Square), [b_QTf], [b_sqQ])
            for t in (range(4) if stage >= 3 else []):
                tsl = slice(t * 128, (t + 1) * 128)
                blkq = b0 + t // 2
                gps, bgp = gp_ring.next()
                pe(lambda e: e.matmul(gps[:, 0:1], lhsT=sqQ[:, tsl], rhs=C["onesF"][:, 0:1], start=True, stop=True),
                   [b_sqQ], [bgp])
                dve(lambda e: e.tensor_scalar(out=st[:, 27:28], in0=gps[:, 0:1], scalar1=cst[:, 3:4], scalar2=1e-30,
                                              op0=ALU.mult, op1=ALU.add), [bgp, b_cst], [b_st])
                act(lambda e: e.activation(out=st[:, 27:28], in_=st[:, 27:28], func=AF.Ln), [b_st], [b_st])
                act(lambda e: e.activation(out=st[:, 27:28], in_=st[:, 27:28], func=AF.Exp, scale=0.5), [b_st], [b_st])
                dve(lambda e: e.tensor_tensor(out=st[:, 27:28], in0=st[:, 27:28], in1=C["slopeT"][:, t:t + 1],
                                              op=ALU.add), [b_st], [b_st])
                dve(lambda e: e.tensor_scalar(out=st[:, 27:28], in0=st[:, 27:28], scalar1=-1.0, scalar2=NEG,
                                              op0=ALU.mult, op1=ALU.add), [b_st], [b_st])
                gm, bgm = sm_ring.next()
                mk_, bmk = sm_ring.next()
                gps, bgp = gp_ring.next()
                pe(lambda e: e.matmul(gps[:, 0:NB], lhsT=QTf[:, tsl], rhs=kmT[:, 0:NB], start=True, stop=True),
                   [b_QTf, b_km], [bgp])
                dve(lambda e: e.tensor_scalar(out=mk_[:, 0:NB], in0=C["jrow"][:, 0:NB], scalar1=float(blkq), scalar2=None,
                                              op0=ALU.is_lt), [], [bmk])
                dve(lambda e: e.tensor_tensor(out=gm[:, 0:NB], in0=gps[:, 0:NB], in1=mk_[:, 0:NB], op=ALU.mult),
                    [bgp, bmk], [bgm])
                dve(lambda e: e.tensor_scalar(out=mk_[:, 64:64 + NB], in0=mk_[:, 0:NB], scalar1=1e30, scalar2=-1e30,
                                              op0=ALU.mult, op1=ALU.add), [bmk], [bmk])
                dve(lambda e: e.tensor_tensor(out=gm[:, 0:NB], in0=gm[:, 0:NB], in1=mk_[:, 64:64 + NB], op=ALU.add),
                    [bgm, bmk], [bgm])
                dve(lambda e: e.max(out=st[:, 0:8], in_=gm[:, 0:NB]), [bgm], [b_st])
                dve(lambda e: e.tensor_scalar(out=gm[:, 0:NB], in0=gm[:, 0:NB], scalar1=st[:, 2:3], scalar2=None,
                                              op0=ALU.is_ge), [bgm, b_st], [bgm])
                dve(lambda e: e.tensor_tensor(out=gm[:, 0:NB], in0=gm[:, 0:NB], in1=mk_[:, 0:NB], op=ALU.mult),
                    [bgm, bmk], [bgm])
                dve(lambda e: e.tensor_scalar(out=mk_[:, 0:NB], in0=C["jrow"][:, 0:NB], scalar1=float(blkq), scalar2=None,
                                              op0=ALU.is_equal), [bmk], [bmk])
                dve(lambda e: e.tensor_tensor(out=gm[:, 0:NB], in0=gm[:, 0:NB], in1=mk_[:, 0:NB], op=ALU.add),
                    [bgm, bmk], [bgm])
                bq, bbq = sm_ring.next()
                dve(lambda e: e.tensor_scalar(out=gm[:, 64:64 + NB], in0=gm[:, 0:NB], scalar1=-NEG, scalar2=st[:, 27:28],
                                              op0=ALU.mult, op1=ALU.add), [bgm, b_st], [bgm])
                dve(lambda e: e.tensor_copy(out=xn[:, 0:NB], in_=gm[:, 64:64 + NB]), [bgm], [b_xn])
                pe(lambda e: e.transpose(out=tpx[0:NB, 0, :], in_=xn[:, 0:NB], identity=C["identB"][:]),
                   [b_xn], [b_tpx])
                evac(biasT[0:NB, tsl], tpx[0:NB, 0, :], [b_tpx], [b_bT])
            nkt = gt0 + 4 if stage >= 3 else 0
            for kt in range(nkt):
                ktl = kt - gt0
                jb = kt // 2
                ps, bp = acc_ring.next()
                pe(lambda e: e.matmul(ps[:], lhsT=KT_all[:, kt * 128:(kt + 1) * 128], rhs=QT[:], start=True, stop=False),
                   [b_KT, b_QT], [bp])
                r0 = 32 * (jb // 32)
                pe(lambda e: e.matmul(ps[:], lhsT=C["sel32"][r0:r0 + KBK, (jb % 32) * 128:(jb % 32 + 1) * 128],
                                      rhs=biasT[r0:r0 + KBK, :], start=False, stop=(ktl < 0)), [b_bT], [bp])
                if ktl >= 0:
                    pe(lambda e: e.matmul(ps[:], lhsT=C["identB"][:], rhs=C["cmask"][:, ktl * 512:(ktl + 1) * 512],
                                          start=False, stop=True), [], [bp])
                pt, bpt = pt_ring.next()
                ai = 3 - ktl
                act(lambda e: e.activation(out=pt[:], in_=ps[:], func=AF.Exp, bias=C["alibiT"][:, ai:ai + 1]),
                    [bp], [bpt])
                for qt in range(4):
                    if ktl > qt:
                        continue
                    last_kt = gt0 + qt
                    pe(lambda e: e.matmul(o_ps(qt), lhsT=pt[:, qt * 128:(qt + 1) * 128], rhs=V_all[:, kt, 0:129],
                                          start=(kt == 0 and qt % 2 == 0), stop=(kt == last_kt),
                                          skip_group_check=True), [bpt, b_V], [b_ops[qt]])
            for qt in range(4):
                if stage >= 3:
                    dve(lambda e: e.reciprocal(out=st[:, 28:29], in_=o_ps(qt)[:, 128:129]), [b_ops[qt]], [b_st])
                if stage >= 3:
                    dve(lambda e: e.tensor_scalar(out=ybuf[qt][:, 128:256], in0=o_ps(qt)[:, 0:128], scalar1=st[:, 28:29],
                                              scalar2=None, op0=ALU.mult), [b_ops[qt], b_st], [b_y[qt]])
                out_toks.append(k.dma("sp", d_y[qt], y_d[n0 + qt * 128:n0 + (qt + 1) * 128, :], ybuf[qt][:],
                                      reads=[b_y[qt]], force=True))
    k.finish(out_toks)
    print("build_a nops", k.nops, flush=True)
    k.close()
    return nc


def inputs_a(T, core, x2d, w_in, conv_w, a_log, dt_bias, gdn_norm_w, pre_mix_norm):
    c = core
    cols = np.concatenate([np.arange(c * 128, c * 128 + 128), 1024 + np.arange(c * 128, c * 128 + 128),
                           2048 + np.arange(c * 128, c * 128 + 128), 4112 + np.arange(c * 128, c * 128 + 128),
                           5136 + np.arange(c * 128, c * 128 + 128), 3072 + np.arange(c * 128, c * 128 + 128),
                           6160 + np.arange(c * 128, c * 128 + 128), [4096 + c], [4104 + c]]).astype(np.int64)
    d = consts_a(T, c)
    d["x"] = x2d
    d["w_in"] = np.ascontiguousarray(w_in[:, cols])
    cw = np.zeros((128, 16), np.float32)
    for ci in range(3):
        for j in range(4):
            cw[:, ci * 4 + j] = conv_w[j, ci * 1024 + c * 128:ci * 1024 + (c + 1) * 128]
    d["convw"] = cw
    d["alog"] = np.full((128, 16), a_log[c], np.float32)
    d["dtb"] = np.full((128, 16), dt_bias[c], np.float32)
    d["gnw"] = _rep(gdn_norm_w)
    d["gpre"] = np.ascontiguousarray(np.asarray(pre_mix_norm, np.float32).reshape(NKC, 128).T)
    return d


_NC_CACHE = {}


def kernel(x, mem, pre_mix_norm, w_in, conv_w, a_log, dt_bias, gdn_norm_w, w_out, post_mix_norm,
           pre_mem_norm, mem_kv_norm, w_mq, w_mk, w_mv, w_mo, post_mem_norm,
           pre_mlp_norm, w_up, w_down, post_mlp_norm):
    f32 = lambda a: np.ascontiguousarray(np.asarray(a, dtype=np.float32))
    x = f32(x)
    B, T, _ = x.shape
    N = B * T
    x2d = x.reshape(N, D)
    if ("a", T) not in _NC_CACHE:
        _NC_CACHE[("a", T)] = build_a(T)
    ins_a = [inputs_a(T, c, x2d, f32(w_in)[0], f32(conv_w)[0], f32(a_log)[0], f32(dt_bias)[0], f32(gdn_norm_w)[0],
                      f32(pre_mix_norm)[0]) for c in range(8)]
    res_a = run_bass_kernel_spmd(_NC_CACHE[("a", T)], ins_a, core_ids=list(range(8)))
    y = np.empty((N, D), dtype=ml_dtypes.bfloat16)
    for c in range(8):
        yc = np.asarray(res_a.results[c]["y"])
        y[:, c * 128:(c + 1) * 128] = yc[:, 0:128]
        y[:, 1024 + c * 128:1024 + (c + 1) * 128] = yc[:, 128:256]
    NTOK = N // 8
    if ("b", NTOK) not in _NC_CACHE:
        _NC_CACHE[("b", NTOK)] = build_b(NTOK)
    ident = np.eye(128, dtype=ml_dtypes.bfloat16)
    shared = {"w_out": f32(w_out)[0], "w_mq": f32(w_mq)[0], "w_mk": f32(w_mk)[0], "w_mv": f32(w_mv)[0],
              "w_mo": f32(w_mo)[0], "w_up": f32(w_up)[0], "w_down": f32(w_down)[0], "ident": ident,
              "post_mix_norm": _rep(f32(post_mix_norm)[0]), "pre_mem_norm": _rep(f32(pre_mem_norm)[0]),
              "mem_kv_norm": _rep(f32(mem_kv_norm)[0]), "post_mem_norm": _rep(f32(post_mem_norm)[0]),
              "pre_mlp_norm": _rep(f32(pre_mlp_norm)[0]), "post_mlp_norm": _rep(f32(post_mlp_norm)[0])}
    memf = f32(mem)
    ins_b = []
    for c in range(8):
        d = dict(shared)
        d["x"] = np.ascontiguousarray(x2d[c * NTOK:(c + 1) * NTOK])
        d["y"] = np.ascontiguousarray(y[c * NTOK:(c + 1) * NTOK])
        d["mem"] = np.ascontiguousarray(memf[(c * NTOK) // T])
        ins_b.append(d)
    res_b = run_bass_kernel_spmd(_NC_CACHE[("b", NTOK)], ins_b, core_ids=list(range(8)))
    out = np.concatenate([np.asarray(r["out"]) for r in res_b.results], 0)
    return out.reshape(B, T, D).astype(np.float32)
```

```python
import contextlib
import numpy as np
import ml_dtypes
import concourse.bass as bass
import concourse.mybir as mybir
from concourse.bass_utils import run_bass_kernel_spmd

F32 = mybir.dt.float32
BF16 = mybir.dt.bfloat16
ALU = mybir.AluOpType
AF = mybir.ActivationFunctionType
AX = mybir.AxisListType

D = 2048
NKC = D // 128
DFF = 8192
EPS = 1e-6
NEG = -30000.0


class Buf:
    __slots__ = ("w", "r", "excl")

    def __init__(self, excl=False):
        self.w = None
        self.r = []
        self.excl = excl


def PB():
    return Buf(excl=True)


class KB:
    def __init__(self, nc):
        self.nc = nc
        self.es = contextlib.ExitStack()
        self.E = {"pe": nc.tensor, "act": nc.scalar, "dve": nc.vector, "pool": nc.gpsimd, "sp": nc.sync}
        self.sem = {}
        self.cnt = {}
        for e in ("pe", "act", "dve", "pool"):
            self.sem[e] = self.es.enter_context(nc.semaphore("s_" + e))
            self.cnt[e] = 0
        self.seen = {e: {} for e in self.E}
        self.nbuf = 0
        self.cut = None
        self.nops = 0

    def sb(self, name, shape, dt):
        return self.es.enter_context(self.nc.sbuf_tensor(name, shape, dt))

    def ps(self, name, shape, dt):
        return self.es.enter_context(self.nc.psum_tensor(name, shape, dt))

    def dsem(self, name):
        s = self.es.enter_context(self.nc.semaphore(name))
        self.sem[name] = s
        self.cnt[name] = 0
        return name

    def _wait(self, eng, toks):
        best = {}
        for t in toks:
            if t is None:
                continue
            k, v = t
            if v > best.get(k, 0):
                best[k] = v
        for k, v in best.items():
            if self.seen[eng].get(k, 0) >= v:
                continue
            self.E[eng].wait_ge(self.sem[k], v)
            self.seen[eng][k] = v

    def _deps(self, eng, reads, writes):
        toks = []
        for b in reads:
            toks.append(b.w)
            if b.excl:
                for t in b.r:
                    if t[0] != eng:
                        toks.append(t)
        for b in writes:
            if b.w is not None and b.w[0] != eng:
                toks.append(b.w)
            for t in b.r:
                if t[0] != eng:
                    toks.append(t)
        return toks

    def op(self, eng, fn, reads=(), writes=()):
        self.nops += 1
        if self.cut is not None and self.nops > self.cut:
            return None
        self._wait(eng, self._deps(eng, reads, writes))
        ins = fn(self.E[eng])
        self.cnt[eng] += 1
        tok = (eng, self.cnt[eng])
        ins.then_inc(self.sem[eng], 1)
        for b in reads:
            b.r.append(tok)
        for b in writes:
            b.w = tok
            b.r = []
        return tok

    def dma(self, q, sem, out, in_, reads=(), writes=(), force=False):
        self.nops += 1
        if self.cut is not None and self.nops > self.cut and not force:
            return None
        self._wait(q, self._deps("dma", reads, writes))
        ins = self.E[q].dma_start(out=out, in_=in_)
        self.cnt[sem] += 16
        tok = (sem, self.cnt[sem])
        ins.then_inc(self.sem[sem], 16)
        for b in reads:
            b.r.append(tok)
        for b in writes:
            b.w = tok
            b.r = []
        return tok

    def finish(self, toks):
        self._wait("sp", toks)

    def close(self):
        self.es.close()


class Ring:
    def __init__(self, items):
        self.items = items
        self.i = 0

    def next(self):
        it = self.items[self.i % len(self.items)]
        self.i += 1
        return it


def _rep(v, rows=128):
    v = np.asarray(v, np.float32).reshape(1, -1)
    return np.ascontiguousarray(np.broadcast_to(v, (rows, v.shape[1])))


def build_b(NTOK):
    G = 512
    NG = NTOK // G
    nc = bass.Bass("TRN2", target_bir_lowering=False)
    dr = lambda n, s, dt, kind="ExternalInput": nc.dram_tensor(n, s, dt, kind=kind)
    x_d = dr("x", [NTOK, D], F32)
    y_d = dr("y", [NTOK, D], BF16)
    mem_d = dr("mem", [256, D], F32)
    w_out_d = dr("w_out", [D, D], F32)
    w_mq_d = dr("w_mq", [D, 512], F32)
    w_mk_d = dr("w_mk", [D, 512], F32)
    w_mv_d = dr("w_mv", [D, 512], F32)
    w_mo_d = dr("w_mo", [512, D], F32)
    w_up_d = dr("w_up", [D, DFF], F32)
    w_dn_d = dr("w_down", [DFF, D], F32)
    nrm_d = {n: dr(n, [128, D], F32) for n in
             ("post_mix_norm", "pre_mem_norm", "mem_kv_norm", "post_mem_norm", "pre_mlp_norm", "post_mlp_norm")}
    ident_d = dr("ident", [128, 128], BF16)
    out_d = dr("out", [NTOK, D], F32, kind="ExternalOutput")

    k = KB(nc)
    ident = k.sb("ident_sb", [128, 128], BF16)
    b_ident = Buf()
    xres = k.sb("xres", [128, 4, D], F32)
    b_x = [Buf() for _ in range(4)]
    tmp = k.sb("tmp", [128, 4, D], F32)
    b_tmp = [Buf() for _ in range(4)]
    hT = k.sb("hT", [128, NKC, G], BF16)
    b_hT = Buf()
    actT = k.sb("actT", [128, 32, G], BF16)
    b_actT = Buf()
    wsl = [k.sb(f"wsl{i}", [128, 8192], BF16) for i in range(3)]
    w_ring = Ring([(wsl[i], Buf(), k.dsem(f"d_w{i}")) for i in range(3)])
    nsl = [k.sb(f"nsl{i}", [128, D], F32) for i in range(2)]
    n_ring = Ring([(nsl[i], Buf(), k.dsem(f"d_n{i}")) for i in range(2)])
    ysl = [k.sb(f"ysl{i}", [128, D], BF16) for i in range(1)]
    y_ring = Ring([(ysl[i], Buf(), k.dsem(f"d_y{i}")) for i in range(1)])
    xn = k.sb("xn", [128, D], BF16)
    b_xn = Buf()
    junk, b_junk = xn, b_xn
    stat = k.sb("stat", [128, 16], F32)
    b_stat = Buf()
    relu_s = [k.sb(f"relu{i}", [128, G], F32) for i in range(2)]
    relu_ring = Ring([(relu_s[i], Buf()) for i in range(2)])
    kT = k.sb("kT", [128, 4, 256], BF16)
    b_kT = Buf()
    vS = k.sb("vS", [128, 2, 512], BF16)
    b_vS = Buf()
    qT = k.sb("qT", [128, 4, G], BF16)
    b_qT = Buf()
    oT = k.sb("oT", [128, 4, G], BF16)
    b_oT = Buf()
    pS = [k.sb(f"pS{i}", [128, 256], F32) for i in range(2)]
    p_ring = Ring([(pS[i], Buf()) for i in range(2)])
    pB = [k.sb(f"pB{i}", [128, 256], BF16) for i in range(2)]
    pb_ring = Ring([(pB[i], Buf()) for i in range(2)])
    pT = [k.sb(f"pT{i}", [128, 2, 128], BF16) for i in range(2)]
    pt_ring = Ring([(pT[i], Buf()) for i in range(2)])
    d_xs = [k.dsem(f"d_x{i}") for i in range(4)]
    d_c = k.dsem("d_c")
    d_o = k.dsem("d_o")
    acc = [k.ps(f"acc{i}", [128, 512], F32) for i in range(4)]
    acc_ring = Ring([(acc[i], PB()) for i in range(4)])
    tp = [k.ps(f"tp{i}", [128, 8, 128], BF16) for i in range(2)]
    tp_ring = Ring([(tp[i], PB()) for i in range(2)])
    sc = [k.ps(f"sc{i}", [128, 512], F32) for i in range(2)]
    sc_ring = Ring([(sc[i], PB()) for i in range(2)])

    k.dma("sp", d_c, ident[:], ident_d[:, :], writes=[b_ident])

    evac_i = [0]

    def evac_copy(out_ap, in_ap, reads, writes):
        evac_i[0] += 1
        if evac_i[0] % 2:
            return k.op("act", lambda e: e.activation(out=out_ap, in_=in_ap, func=AF.Copy), reads, writes)
        return k.op("dve", lambda e: e.tensor_copy(out=out_ap, in_=in_ap), reads, writes)

    wplan = []
    wplan.append((w_mk_d[:, :], NKC, 512))
    wplan.append((w_mv_d[:, :], NKC, 512))
    for _g in range(NG):
        for cg in range(4):
            wplan.append((w_out_d[:, cg * 512:(cg + 1) * 512], NKC, 512))
        wplan.append((w_mq_d[:, :], NKC, 512))
        for cg in range(4):
            wplan.append((w_mo_d[:, cg * 512:(cg + 1) * 512], 4, 512))
        for half in range(2):
            for fg in range(8):
                c0 = half * 4096 + fg * 512
                wplan.append((w_up_d[:, c0:c0 + 512], NKC, 512))
            for cg in range(8):
                wplan.append((w_dn_d[half * 4096:(half + 1) * 4096, cg * 256:(cg + 1) * 256], 32, 256))
    wstate = {"issued": 0, "used": 0, "q": []}

    def _issue_w():
        if wstate["issued"] >= len(wplan):
            return
        src_ap, nk, ncol = wplan[wstate["issued"]]
        wstate["issued"] += 1
        sl, b, sem = w_ring.next()
        view = sl[:, 0:nk * ncol].rearrange("p (k c) -> p k c", k=nk)
        k.dma("pool", sem, view, src_ap.rearrange("(k p) c -> p k c", p=128), writes=[b])
        wstate["q"].append((view, b, nk, ncol))

    def load_w(src_ap, nk, ncol):
        while wstate["issued"] < wstate["used"] + 3:
            if wstate["issued"] >= len(wplan):
                break
            _issue_w()
        view, b, pk, pc = wstate["q"].pop(0)
        assert (pk, pc) == (nk, ncol), (pk, pc, nk, ncol)
        wstate["used"] += 1
        return view, b

    def load_nrm(name):
        sl, b, sem = n_ring.next()
        k.dma("sp", sem, sl[:], nrm_d[name][:, :], writes=[b])
        return sl, b

    def rstd_of(src_ap, b_src, col, from_psum=False):
        k.op("act", lambda e: e.activation(out=junk[:], in_=src_ap, func=AF.Square,
                                           accum_out=stat[:, col:col + 1]),
             [b_src], [b_junk, b_stat])
        k.op("act", lambda e: e.activation(out=stat[:, col:col + 1], in_=stat[:, col:col + 1], func=AF.Ln,
                                           scale=1.0 / D, bias=eps_c[:, 0:1]), [b_stat, b_eps], [b_stat])
        k.op("act", lambda e: e.activation(out=stat[:, col:col + 1], in_=stat[:, col:col + 1], func=AF.Exp,
                                           scale=-0.5), [b_stat], [b_stat])

    eps_c = k.sb("eps_c", [128, 1], F32)
    b_eps = Buf()
    k.op("dve", lambda e: e.memset(eps_c[:], EPS), [], [b_eps])

    def transpose_tile(src_bf, b_src, t):
        for h in range(2):
            ps, bp = tp_ring.next()
            for j in range(8):
                kc = h * 8 + j
                k.op("pe", lambda e: e.transpose(out=ps[:, j, :], in_=src_bf[:, kc * 128:(kc + 1) * 128],
                                                 identity=ident[:]),
                     [b_src, b_ident], [bp])
            evac_copy(hT[:, h * 8:(h + 1) * 8, t * 128:(t + 1) * 128], ps[:], [bp], [b_hT])

    def norm_to_hT(t, gname_sl, b_g):
        rstd_of(xres[:, t, :], b_x[t], t)
        k.op("dve", lambda e: e.scalar_tensor_tensor(out=xn[:], in0=xres[:, t, :], scalar=stat[:, t:t + 1],
                                                     in1=gname_sl[:], op0=ALU.mult, op1=ALU.mult),
             [b_x[t], b_stat, b_g], [b_xn])
        transpose_tile(xn, b_xn, t)

    def post_norm_residual(t, g_sl, b_g):
        rstd_of(tmp[:, t, :], b_tmp[t], 8 + t)
        k.op("dve", lambda e: e.scalar_tensor_tensor(out=tmp[:, t, :], in0=tmp[:, t, :],
                                                     scalar=stat[:, 8 + t:9 + t], in1=g_sl[:],
                                                     op0=ALU.mult, op1=ALU.mult),
             [b_tmp[t], b_stat, b_g], [b_tmp[t]])
        k.op("dve", lambda e: e.tensor_tensor(out=xres[:, t, :], in0=xres[:, t, :], in1=tmp[:, t, :], op=ALU.add),
             [b_x[t], b_tmp[t]], [b_x[t]])

    def proj_tokmajor(w_src, K, lhs, b_lhs, first=True, ncol=512):
        for cg in range(D // ncol):
            wv, bw = load_w(w_src[:, cg * ncol:(cg + 1) * ncol], K, ncol)
            for t in range(4):
                ps, bp = acc_ring.next()
                for kc in range(K):
                    k.op("pe", lambda e: e.matmul(ps[:, 0:ncol], lhsT=lhs(kc, t), rhs=wv[:, kc, :],
                                                  start=(kc == 0), stop=(kc == K - 1)),
                         [b_lhs, bw], [bp])
                dst = tmp[:, t, cg * ncol:(cg + 1) * ncol]
                if first:
                    evac_copy(dst, ps[:, 0:ncol], [bp], [b_tmp[t]])
                else:
                    k.op("dve", lambda e: e.tensor_tensor(out=dst, in0=dst, in1=ps[:, 0:ncol], op=ALU.add),
                         [bp, b_tmp[t]], [b_tmp[t]])

    gkv, b_gkv = load_nrm("mem_kv_norm")
    for mt in range(2):
        k.dma("sp", d_xs[mt], xres[:, mt, :], mem_d[mt * 128:(mt + 1) * 128, :], writes=[b_x[mt]])
        rstd_of(xres[:, mt, :], b_x[mt], mt)
        k.op("dve", lambda e: e.scalar_tensor_tensor(out=xn[:], in0=xres[:, mt, :], scalar=stat[:, mt:mt + 1],
                                                     in1=gkv[:], op0=ALU.mult, op1=ALU.mult),
             [b_x[mt], b_stat, b_gkv], [b_xn])
        transpose_tile(xn, b_xn, mt)
    wv_, bw_ = load_w(w_mk_d[:, :], NKC, 512)
    for h in range(4):
        ps, bp = acc_ring.next()
        for kc in range(NKC):
            k.op("pe", lambda e: e.matmul(ps[:, 0:256], lhsT=wv_[:, kc, h * 128:(h + 1) * 128], rhs=hT[:, kc, 0:256],
                                          start=(kc == 0), stop=(kc == NKC - 1)), [b_hT, bw_], [bp])
        evac_copy(kT[:, h, :], ps[:, 0:256], [bp], [b_kT])
    wv_, bw_ = load_w(w_mv_d[:, :], NKC, 512)
    for mt in range(2):
        ps, bp = acc_ring.next()
        for kc in range(NKC):
            k.op("pe", lambda e: e.matmul(ps[:], lhsT=hT[:, kc, mt * 128:(mt + 1) * 128], rhs=wv_[:, kc, :],
                                          start=(kc == 0), stop=(kc == NKC - 1)), [b_hT, bw_], [bp])
        evac_copy(vS[:, mt, :], ps[:], [bp], [b_vS])

    out_toks = []
    for g in range(NG):
        t0 = g * G
        for t in range(4):
            k.dma("sp", d_xs[t], xres[:, t, :], x_d[t0 + t * 128:t0 + (t + 1) * 128, :], writes=[b_x[t]])
        for t in range(4):
            ys, by, sem = y_ring.next()
            k.dma("sp", sem, ys[:], y_d[t0 + t * 128:t0 + (t + 1) * 128, :], writes=[by])
            transpose_tile(ys, by, t)
        proj_tokmajor(w_out_d, NKC, lambda kc, t: hT[:, kc, t * 128:(t + 1) * 128], b_hT)
        gs, bg = load_nrm("post_mix_norm")
        for t in range(4):
            post_norm_residual(t, gs, bg)
        gs, bg = load_nrm("pre_mem_norm")
        for t in range(4):
            norm_to_hT(t, gs, bg)
        wq, bwq = load_w(w_mq_d[:, :], NKC, 512)
        for h in range(4):
            ps, bp = acc_ring.next()
            for kc in range(NKC):
                k.op("pe", lambda e: e.matmul(ps[:], lhsT=wq[:, kc, h * 128:(h + 1) * 128], rhs=hT[:, kc, :],
                                              start=(kc == 0), stop=(kc == NKC - 1)), [b_hT, bwq], [bp])
            k.op("act", lambda e: e.activation(out=qT[:, h, :], in_=ps[:], func=AF.Copy, scale=128.0 ** -0.5),
                 [bp], [b_qT])
        for t in range(4):
            for h in range(4):
                s_ps, bs = sc_ring.next()
                k.op("pe", lambda e: e.matmul(s_ps[:, 0:256], lhsT=qT[:, h, t * 128:(t + 1) * 128], rhs=kT[:, h, :],
                                              start=True, stop=True), [b_qT, b_kT], [bs])
                c0 = 12 + (h % 2) * 2
                k.op("dve", lambda e: e.tensor_reduce(out=stat[:, c0:c0 + 1], in_=s_ps[:, 0:256], axis=AX.X,
                                                      op=ALU.max, negate=True), [bs], [b_stat])
                p_s, bps = p_ring.next()
                k.op("act", lambda e: e.activation(out=p_s[:], in_=s_ps[:, 0:256], func=AF.Exp,
                                                   bias=stat[:, c0:c0 + 1], accum_out=stat[:, c0 + 1:c0 + 2]),
                     [bs, b_stat], [bps, b_stat])
                k.op("dve", lambda e: e.reciprocal(out=stat[:, c0 + 1:c0 + 2], in_=stat[:, c0 + 1:c0 + 2]),
                     [b_stat], [b_stat])
                p_b, bpb = pb_ring.next()
                k.op("dve", lambda e: e.tensor_scalar(out=p_b[:], in0=p_s[:], scalar1=stat[:, c0 + 1:c0 + 2],
                                                      scalar2=None, op0=ALU.mult), [bps, b_stat], [bpb])
                tps, btp = tp_ring.next()
                for mt in range(2):
                    k.op("pe", lambda e: e.transpose(out=tps[:, mt, :], in_=p_b[:, mt * 128:(mt + 1) * 128],
                                                     identity=ident[:]), [bpb, b_ident], [btp])
                p_t, bpt = pt_ring.next()
                evac_copy(p_t[:], tps[:, 0:2, :], [btp], [bpt])
                o_ps, bo = sc_ring.next()
                for mt in range(2):
                    k.op("pe", lambda e: e.matmul(o_ps[:, 0:128], lhsT=vS[:, mt, h * 128:(h + 1) * 128],
                                                  rhs=p_t[:, mt, :], start=(mt == 0), stop=(mt == 1)),
                         [b_vS, bpt], [bo])
                evac_copy(oT[:, h, t * 128:(t + 1) * 128], o_ps[:, 0:128], [bo], [b_oT])
        proj_tokmajor(w_mo_d, 4, lambda kc, t: oT[:, kc, t * 128:(t + 1) * 128], b_oT)
        gs, bg = load_nrm("post_mem_norm")
        for t in range(4):
            post_norm_residual(t, gs, bg)
        gs, bg = load_nrm("pre_mlp_norm")
        for t in range(4):
            norm_to_hT(t, gs, bg)
        for half in range(2):
            for fg in range(8):
                c0 = half * 4096 + fg * 512
                wu, bwu = load_w(w_up_d[:, c0:c0 + 512], NKC, 512)
                for j in range(4):
                    ps, bp = acc_ring.next()
                    for kc in range(NKC):
                        k.op("pe", lambda e: e.matmul(ps[:], lhsT=wu[:, kc, j * 128:(j + 1) * 128], rhs=hT[:, kc, :],
                                                      start=(kc == 0), stop=(kc == NKC - 1)), [b_hT, bwu], [bp])
                    rs, br = relu_ring.next()
                    k.op("act", lambda e: e.activation(out=rs[:], in_=ps[:], func=AF.Relu), [bp], [br])
                    fi = fg * 4 + j
                    k.op("dve", lambda e: e.tensor_tensor(out=actT[:, fi, :], in0=rs[:], in1=rs[:], op=ALU.mult),
                         [br], [b_actT])
            r0 = half * 4096
            proj_tokmajor(w_dn_d[r0:r0 + 4096, :], 32, lambda kc, t: actT[:, kc, t * 128:(t + 1) * 128], b_actT,
                          first=(half == 0), ncol=256)
        gs, bg = load_nrm("post_mlp_norm")
        for t in range(4):
            post_norm_residual(t, gs, bg)
            out_toks.append(k.dma("sp", d_o, out_d[t0 + t * 128:t0 + (t + 1) * 128, :], xres[:, t, :],
                                  reads=[b_x[t]]))
    k.finish(out_toks)
    k.close()
    return nc


def consts_a(T, core):
    NB = T // 256
    c = {}
    c["identF"] = np.eye(128, dtype=np.float32)
    c["identB"] = np.eye(128, dtype=ml_dtypes.bfloat16)
    r = np.arange(128)
    same = (r[:, None] // 64) == (r[None, :] // 64)
    c["U"] = (same & (r[:, None] <= r[None, :])).astype(np.float32)
    c["Ls"] = (same & (r[:, None] > r[None, :])).astype(np.float32)
    c["blk"] = same.astype(np.float32)
    c["onesF"] = np.ones((128, 128), np.float32)
    c["halfm"] = np.zeros((128, 16), np.float32)
    c["halfm"][:, 0] = r < 64
    c["halfm"][:, 1] = r >= 64
    c["jrow"] = _rep(np.arange(64, dtype=np.float32))
    sel = np.zeros((128, 32, 128), np.float32)
    for j in range(32):
        sel[j, j, :] = 1.0
        sel[32 + j, j, :] = 1.0
    c["sel32"] = sel.reshape(128, 32 * 128).astype(ml_dtypes.bfloat16)
    cm = np.zeros((128, 4, 512), np.float32)
    qi = np.arange(512)
    for ktl in range(4):
        key = ktl * 128 + r
        cm[:, ktl, :] = np.where(key[:, None] <= qi[None, :], 0.0, NEG)
    c["cmask"] = cm.reshape(128, 2048).astype(ml_dtypes.bfloat16)
    slope = float(2.0 ** (-8.0 * (core + 1) / 8.0))
    ntile = T // 128
    i = np.arange(ntile)
    c["alibiT"] = (slope * (r[:, None] + 128.0 * (3 - i[None, :]))).astype(np.float32)
    c["slopeT"] = (slope * (r[:, None] + 128.0 * np.arange(16)[None, :])).astype(np.float32)
    return c


CONST_A = {"identF": ([128, 128], F32), "identB": ([128, 128], BF16), "U": ([128, 128], F32),
           "Ls": ([128, 128], F32), "blk": ([128, 128], F32), "onesF": ([128, 128], F32),
           "halfm": ([128, 16], F32), "sel32": ([128, 4096], BF16), "cmask": ([128, 2048], BF16),
           "slopeT": ([128, 16], F32), "convw": ([128, 16], F32), "alog": ([128, 16], F32),
           "dtb": ([128, 16], F32), "gnw": ([128, 128], F32), "gpre": ([128, NKC], F32)}


def build_a(T, stage=3, cut=None):
    G = 512
    NG = T // G
    NB = T // 256
    NT = T // 128
    KBK = min(32, NB)
    N = 2 * T
    NW = 898
    nc = bass.Bass("TRN2", target_bir_lowering=False)
    dr = lambda n, s, dt, kind="ExternalInput": nc.dram_tensor(n, s, dt, kind=kind)
    x_d = dr("x", [N, D], F32)
    w_d = dr("w_in", [D, NW], F32)
    cd = {n: dr(n, sh, dt) for n, (sh, dt) in CONST_A.items()}
    cd["jrow"] = dr("jrow", [128, 64], F32)
    cd["alibiT"] = dr("alibiT", [128, NT], F32)
    y_d = dr("y", [N, 256], BF16, kind="ExternalOutput")

    k = KB(nc)
    k.cut = cut
    C = {}
    bC = Buf()
    d_c = k.dsem("d_c")
    for n in cd:
        sh = list(cd[n].shape)
        C[n] = k.sb("c_" + n, sh, cd[n].dtype)
        k.dma("sp", d_c, C[n][:], cd[n][:, :], writes=[bC])
    k.finish([bC.w])
    for e in ("pe", "act", "dve"):
        k._wait(e, [bC.w])
    bC = Buf()

    w_sb = k.sb("w_sb", [128, NKC, 960], BF16)
    b_w = Buf()
    wtmp = [k.sb(f"wtmp{i}", [128, NW], F32) for i in range(2)]
    wt_ring = Ring([(wtmp[i], Buf(), k.dsem(f"d_wt{i}")) for i in range(2)])
    KT_all = k.sb("KT_all", [128, T], BF16)
    b_KT = Buf()
    V_all = k.sb("V_all", [128, NT, 136], BF16)
    b_V = Buf()
    kmT = k.sb("kmT", [128, NB], F32)
    b_km = Buf()
    xt = [k.sb(f"xt{i}", [128, D], F32) for i in range(2)]
    x_ring = Ring([(xt[i], Buf(), k.dsem(f"d_xa{i}")) for i in range(2)])
    xn = k.sb("xn_a", [128, D], BF16)
    b_xn = Buf()
    hT = k.sb("hT_a", [128, NKC, G], BF16)
    b_hT = Buf()
    st = k.sb("st_a", [128, 32], F32)
    b_st = Buf()
    cst = k.sb("cst_a", [128, 8], F32)
    b_cst = Buf()
    pre = k.sb("pre", [128, 3, 515], F32)
    b_pre = Buf()
    cs = k.sb("cs", [128, 3, G], F32)
    b_cs = Buf()
    cvt = k.sb("cvt", [128, G], F32)
    b_cvt = Buf()
    rn = k.sb("rn", [128, G], F32)
    b_rn = Buf()
    QnT = k.sb("QnT", [128, G], F32)
    b_Qn = Buf()
    KnT = k.sb("KnT", [128, G], F32)
    b_Kn = Buf()
    zs = k.sb("zs", [128, 4, 128], F32)
    b_zs = [Buf() for _ in range(4)]
    ab = k.sb("ab", [128, 4, 2], F32)
    b_ab = [Buf() for _ in range(4)]
    Sst = [k.sb(f"S{i}", [128, 128], F32) for i in range(2)]
    b_S = [Buf(), Buf()]
    ybuf = [k.sb(f"ybuf{i}", [128, 256], BF16) for i in range(4)]
    b_y = [Buf() for _ in range(4)]
    d_y = [k.dsem(f"d_yo{i}") for i in range(4)]
    QT = k.sb("QT", [128, G], BF16)
    b_QT = Buf()
    QTf = k.sb("QTf", [128, G], F32)
    b_QTf = Buf()
    sqQ = k.sb("sqQ", [128, G], F32)
    b_sqQ = Buf()
    biasT = k.sb("biasT", [64, G], BF16)
    b_bT = Buf()
    PTs = [k.sb(f"PT{i}", [128, G], BF16) for i in range(3)]
    pt_ring = Ring([(PTs[i], Buf()) for i in range(3)])
    sm = [k.sb(f"sm{i}", [128, 128], F32) for i in range(10)]
    sm_ring = Ring([(sm[i], Buf()) for i in range(10)])
    smm = [k.sb(f"smm{i}", [128, 128], F32) for i in range(3)]
    smm_ring = Ring([(smm[i], Buf()) for i in range(3)])
    stm = k.sb("stm_a", [128, 32], F32)
    b_stm = Buf()
    named = {}

    def nb(name, par):
        key = (name, par)
        if key not in named:
            named[key] = (k.sb(f"n_{name}{par}", [128, 128], F32), Buf())
        return named[key]

    acc = [k.ps(f"a_acc{i}", [128, 512], F32) for i in range(3)]
    acc_ring = Ring([(acc[i], PB()) for i in range(3)])
    tpx = k.ps("a_tp", [128, 8, 128], BF16)
    b_tpx = PB()
    gp = [k.ps(f"a_gp{i}", [128, 4, 128], F32) for i in range(2)]
    _gpb = [PB(), PB()]
    gp_ring = Ring([(gp[i % 2][:, i // 2, :], _gpb[i % 2]) for i in range(8)])
    ops_ = [k.ps(f"a_o{i}", [128, 2, 129], F32) for i in range(2)]
    _ob = [PB(), PB()]
    b_ops = [_ob[0], _ob[0], _ob[1], _ob[1]]

    def o_ps(qt):
        return ops_[qt // 2][:, qt % 2, :]

    ev = [0]

    def evac(out_ap, in_ap, reads, writes):
        ev[0] += 1
        if ev[0] % 2:
            return k.op("act", lambda e: e.activation(out=out_ap, in_=in_ap, func=AF.Copy), reads, writes)
        return k.op("dve", lambda e: e.tensor_copy(out=out_ap, in_=in_ap), reads, writes)

    def dve(fn, reads, writes):
        return k.op("dve", fn, reads, writes)

    def act(fn, reads, writes):
        return k.op("act", fn, reads, writes)

    def pe(fn, reads, writes):
        return k.op("pe", fn, reads, writes)

    dve(lambda e: e.memset(cst[:, 0:1], EPS), [], [b_cst])
    dve(lambda e: e.memset(cst[:, 1:2], 1.0), [], [b_cst])
    act(lambda e: e.activation(out=cst[:, 2:3], in_=C["alog"][:, 0:1], func=AF.Exp), [b_cst], [b_cst])
    dve(lambda e: e.tensor_scalar(out=cst[:, 2:3], in0=cst[:, 2:3], scalar1=-1.0, scalar2=None, op0=ALU.mult),
        [b_cst], [b_cst])
    dve(lambda e: e.memset(cst[:, 4:5], 128.0 * EPS), [], [b_cst])
    dve(lambda e: e.memset(V_all[:], 1.0), [], [b_V])
    for kc in range(NKC):
        wt, bwt, sem = wt_ring.next()
        k.dma("sp", sem, wt[:], w_d[kc * 128:(kc + 1) * 128, :], writes=[bwt])
        dve(lambda e: e.tensor_scalar(out=w_sb[:, kc, 0:NW], in0=wt[:], scalar1=C["gpre"][:, kc:kc + 1], scalar2=None,
                                      op0=ALU.mult), [bwt], [b_w])

    def rstd_ln_exp(col_ap, scale, bias_col):
        act(lambda e: e.activation(out=col_ap, in_=col_ap, func=AF.Ln, scale=scale, bias=bias_col),
            [b_st, b_cst], [b_st])
        act(lambda e: e.activation(out=col_ap, in_=col_ap, func=AF.Exp, scale=-0.5), [b_st], [b_st])

    out_toks = []
    for b in range(2):
        dve(lambda e: e.memset(pre[:], 0.0), [], [b_pre])
        dve(lambda e: e.memset(Sst[0][:], 0.0), [], [b_S[0]])
        dve(lambda e: e.memset(cst[:, 3:4], 0.0), [], [b_cst])
        dve(lambda e: e.memset(kmT[:], 0.0), [], [b_km])
        scur = 0
        for g in range(NG):
            t0 = g * G
            n0 = b * T + t0
            gt0 = t0 // 128
            b0 = t0 // 256
            for t in (range(4) if stage >= 1 else []):
                xs, bx, sem = x_ring.next()
                k.dma("sp", sem, xs[:], x_d[n0 + t * 128:n0 + (t + 1) * 128, :], writes=[bx])
                act(lambda e: e.activation(out=xn[:], in_=xs[:], func=AF.Square, accum_out=st[:, t:t + 1]),
                    [bx], [b_xn, b_st])
                rstd_ln_exp(st[:, t:t + 1], 1.0 / D, cst[:, 0:1])
                dve(lambda e: e.tensor_scalar(out=xn[:], in0=xs[:], scalar1=st[:, t:t + 1], scalar2=None,
                                              op0=ALU.mult), [bx, b_st], [b_xn])
                for h in range(2):
                    for j in range(8):
                        kc = h * 8 + j
                        pe(lambda e: e.transpose(out=tpx[:, j, :], in_=xn[:, kc * 128:(kc + 1) * 128],
                                                 identity=C["identB"][:]), [b_xn], [b_tpx])
                    evac(hT[:, h * 8:(h + 1) * 8, t * 128:(t + 1) * 128], tpx[:], [b_tpx], [b_hT])
            for ci in (range(5) if stage >= 1 else []):
                ps, bp = acc_ring.next()
                for kc in range(NKC):
                    pe(lambda e: e.matmul(ps[:], lhsT=w_sb[:, kc, ci * 128:(ci + 1) * 128], rhs=hT[:, kc, :],
                                          start=(kc == 0), stop=(kc == NKC - 1)), [b_w, b_hT], [bp])
                if ci < 3:
                    evac(pre[:, ci, 3:515], ps[:], [bp], [b_pre])
                elif ci == 3:
                    act(lambda e: e.activation(out=QT[:], in_=ps[:], func=AF.Copy, scale=128.0 ** -0.5),
                        [bp], [b_QT])
                    dve(lambda e: e.tensor_scalar(out=QTf[:], in0=ps[:], scalar1=128.0 ** -0.5, scalar2=None,
                                                  op0=ALU.mult), [bp], [b_QTf])
                else:
                    act(lambda e: e.activation(out=KT_all[:, t0:t0 + G], in_=ps[:], func=AF.Copy), [bp], [b_KT])
                    dve(lambda e: e.tensor_reduce(out=kmT[:, b0:b0 + 2],
                                                  in_=ps[:].rearrange("p (a c) -> p a c", a=2),
                                                  axis=AX.X, op=ALU.add), [bp], [b_km])
                    act(lambda e: e.activation(out=cvt[:], in_=ps[:], func=AF.Square), [bp], [b_cvt])
                    p2, bp2 = acc_ring.next()
                    pe(lambda e: e.matmul(p2[:], lhsT=C["onesF"][:], rhs=cvt[:], start=True, stop=True),
                       [b_cvt], [bp2])
                    dve(lambda e: e.tensor_reduce(out=st[:, 8:9], in_=p2[:], axis=AX.X, op=ALU.max), [bp2], [b_st])
                    dve(lambda e: e.tensor_tensor(out=cst[:, 3:4], in0=cst[:, 3:4], in1=st[:, 8:9], op=ALU.max),
                        [b_st, b_cst], [b_cst])
            for t in (range(4) if stage >= 1 else []):
                ps, bp = acc_ring.next()
                for kc in range(NKC):
                    pe(lambda e: e.matmul(ps[:, 0:258], lhsT=hT[:, kc, t * 128:(t + 1) * 128], rhs=w_sb[:, kc, 640:898],
                                          start=(kc == 0), stop=(kc == NKC - 1)), [b_w, b_hT], [bp])
                act(lambda e: e.activation(out=zs[:, t, :], in_=ps[:, 0:128], func=AF.Silu), [bp], [b_zs[t]])
                dve(lambda e: e.tensor_copy(out=V_all[:, gt0 + t, 0:128], in_=ps[:, 128:256]), [bp], [b_V])
                dve(lambda e: e.tensor_copy(out=ab[:, t, :], in_=ps[:, 256:258]), [bp], [b_ab[t]])

            if stage < 3:
                for t in range(4):
                    dve(lambda e: e.memset(ybuf[t][:], 0.0), [], [b_y[t]])
            for ci in (range(3) if stage >= 2 else []):
                cw = lambda j: C["convw"][:, ci * 4 + j:ci * 4 + j + 1]
                dve(lambda e: e.tensor_scalar(out=cvt[:], in0=pre[:, ci, 0:512], scalar1=cw(0), scalar2=None,
                                              op0=ALU.mult), [b_pre], [b_cvt])
                for j in range(1, 4):
                    dve(lambda e: e.scalar_tensor_tensor(out=cvt[:], in0=pre[:, ci, j:j + 512], scalar=cw(j),
                                                         in1=cvt[:], op0=ALU.mult, op1=ALU.add),
                        [b_pre, b_cvt], [b_cvt])
                act(lambda e: e.activation(out=cs[:, ci, :], in_=cvt[:], func=AF.Silu), [b_cvt], [b_cs])
            dve(lambda e: e.tensor_copy(out=pre[:, :, 0:3], in_=pre[:, :, 512:515]), [b_pre], [b_pre])
            for ci in (range(2) if stage >= 2 else []):
                act(lambda e: e.activation(out=cvt[:], in_=cs[:, ci, :], func=AF.Square), [b_cs], [b_cvt])
                ps, bp = acc_ring.next()
                pe(lambda e: e.matmul(ps[:], lhsT=C["onesF"][:], rhs=cvt[:], start=True, stop=True), [b_cvt], [bp])
                if ci == 0:
                    act(lambda e: e.activation(out=rn[:], in_=ps[:], func=AF.Ln, scale=128.0, bias=cst[:, 4:5]),
                        [bp, b_cst], [b_rn])
                else:
                    act(lambda e: e.activation(out=rn[:], in_=ps[:], func=AF.Ln, scale=1.0, bias=cst[:, 0:1]),
                        [bp, b_cst], [b_rn])
                act(lambda e: e.activation(out=rn[:], in_=rn[:], func=AF.Exp, scale=-0.5), [b_rn], [b_rn])
                dst, bd = (QnT, b_Qn) if ci == 0 else (KnT, b_Kn)
                dve(lambda e: e.tensor_tensor(out=dst[:], in0=cs[:, ci, :], in1=rn[:], op=ALU.mult),
                    [b_cs, b_rn], [bd])

            def gdn_gen():
                nonlocal scur
                for t in (range(4) if stage >= 2 else []):
                    par = t % 2
                    tsl = slice(t * 128, (t + 1) * 128)
                    c0 = 12 + 0
                    act(lambda e: e.activation(out=st[:, 12:13], in_=ab[:, t, 0:1], func=AF.Exp, bias=C["dtb"][:, 0:1]),
                        [b_ab[t]], [b_st])
                    act(lambda e: e.activation(out=st[:, 12:13], in_=st[:, 12:13], func=AF.Ln, bias=cst[:, 1:2]),
                        [b_st, b_cst], [b_st])
                    dve(lambda e: e.tensor_scalar(out=st[:, 12:13], in0=st[:, 12:13], scalar1=cst[:, 2:3], scalar2=None,
                                                  op0=ALU.mult), [b_st, b_cst], [b_st])
                    act(lambda e: e.activation(out=st[:, 13:14], in_=ab[:, t, 1:2], func=AF.Sigmoid), [b_ab[t]], [b_st])
                    dve(lambda e: e.tensor_scalar(out=st[:, 14:16], in0=C["halfm"][:, 0:2], scalar1=st[:, 12:13], scalar2=None,
                                                  op0=ALU.mult), [b_st], [b_st])
                    yield
                    gps, bgp = gp_ring.next()
                    pe(lambda e: e.matmul(gps[:, 0:1], lhsT=C["U"][:], rhs=st[:, 12:13], start=True, stop=True),
                       [b_st], [bgp])
                    pe(lambda e: e.matmul(gps[:, 1:2], lhsT=C["blk"][:], rhs=st[:, 12:13], start=True, stop=True),
                       [b_st], [bgp])
                    pe(lambda e: e.matmul(gps[:, 2:4], lhsT=C["onesF"][:], rhs=st[:, 14:16], start=True, stop=True),
                       [b_st], [bgp])
                    dve(lambda e: e.tensor_copy(out=st[:, 16:20], in_=gps[:, 0:4]), [bgp], [b_st])
                    dve(lambda e: e.tensor_tensor(out=st[:, 20:21], in0=st[:, 17:18], in1=st[:, 16:17], op=ALU.subtract),
                        [b_st], [b_st])
                    act(lambda e: e.activation(out=st[:, 21:22], in_=st[:, 16:17], func=AF.Exp), [b_st], [b_st])
                    act(lambda e: e.activation(out=st[:, 22:23], in_=st[:, 20:21], func=AF.Exp), [b_st], [b_st])
                    act(lambda e: e.activation(out=st[:, 23:25], in_=st[:, 18:20], func=AF.Exp), [b_st], [b_st])
                    dve(lambda e: e.tensor_tensor(out=st[:, 25:26], in0=st[:, 13:14], in1=st[:, 21:22], op=ALU.mult),
                        [b_st], [b_st])
                    g_c, beta_c, egc_c, ekt_c, bg_c = (st[:, 12:13], st[:, 13:14], st[:, 21:22], st[:, 22:23],
                                                       st[:, 25:26])
                    egl = nb("egl", par)
                    dve(lambda e: e.tensor_copy(out=egl[0][:, 0:2], in_=st[:, 23:25]), [b_st], [egl[1]])
                    Kbg, Ktl, Vb = nb("Kbg", par), nb("Ktl", par), nb("Vb", par)
                    yield
                    gps, bgp = gp_ring.next()
                    pe(lambda e: e.transpose(out=gps[:], in_=KnT[:, tsl], identity=C["identF"][:]), [b_Kn], [bgp])
                    dve(lambda e: e.tensor_scalar(out=Kbg[0][:], in0=gps[:], scalar1=bg_c, scalar2=None, op0=ALU.mult),
                        [bgp, b_st], [Kbg[1]])
                    dve(lambda e: e.tensor_scalar(out=Ktl[0][:], in0=gps[:], scalar1=ekt_c, scalar2=None, op0=ALU.mult),
                        [bgp, b_st], [Ktl[1]])
                    yield
                    gps, bgp = gp_ring.next()
                    pe(lambda e: e.transpose(out=gps[:], in_=cs[:, 2, tsl], identity=C["identF"][:]), [b_cs], [bgp])
                    dve(lambda e: e.tensor_scalar(out=Vb[0][:], in0=gps[:], scalar1=beta_c, scalar2=None, op0=ALU.mult),
                        [bgp, b_st], [Vb[1]])
                    Am, bAm = sm_ring.next()
                    dve(lambda e: e.tensor_scalar(out=Am[:], in0=C["U"][:], scalar1=g_c, scalar2=None, op0=ALU.mult),
                        [b_st], [bAm])
                    EL, bEL = sm_ring.next()
                    EU, bEU = sm_ring.next()
                    yield
                    gps, bgp = gp_ring.next()
                    pe(lambda e: e.matmul(gps[:], lhsT=Am[:], rhs=C["Ls"][:], start=True, stop=True), [bAm], [bgp])
                    act(lambda e: e.activation(out=EL[:], in_=gps[:], func=AF.Exp), [bgp], [bEL])
                    dve(lambda e: e.tensor_tensor(out=EL[:], in0=EL[:], in1=C["Ls"][:], op=ALU.mult), [bEL], [bEL])
                    yield
                    gps, bgp = gp_ring.next()
                    pe(lambda e: e.matmul(gps[:], lhsT=C["Ls"][:], rhs=Am[:], start=True, stop=True), [bAm], [bgp])
                    act(lambda e: e.activation(out=EU[:], in_=gps[:], func=AF.Exp), [bgp], [bEU])
                    dve(lambda e: e.tensor_tensor(out=EU[:], in0=EU[:], in1=C["U"][:], op=ALU.mult), [bEU], [bEU])
                    Mm, bM = sm_ring.next()
                    yield
                    gps, bgp = gp_ring.next()
                    pe(lambda e: e.matmul(gps[:], lhsT=KnT[:, tsl], rhs=KnT[:, tsl], start=True, stop=True), [b_Kn], [bgp])
                    dve(lambda e: e.scalar_tensor_tensor(out=Mm[:], in0=gps[:], scalar=beta_c, in1=EL[:],
                                                         op0=ALU.mult, op1=ALU.mult), [bgp, b_st, bEL], [bM])
                    Aq = nb("Aq", par)
                    yield
                    gps, bgp = gp_ring.next()
                    pe(lambda e: e.matmul(gps[:], lhsT=KnT[:, tsl], rhs=QnT[:, tsl], start=True, stop=True),
                       [b_Kn, b_Qn], [bgp])
                    dve(lambda e: e.tensor_tensor(out=Aq[0][:], in0=gps[:], in1=EU[:], op=ALU.mult), [bgp, bEU], [Aq[1]])
                    Nm, bN = sm_ring.next()
                    yield
                    gps, bgp = gp_ring.next()
                    pe(lambda e: e.transpose(out=gps[:], in_=Mm[:], identity=C["identF"][:]), [bM], [bgp])
                    evac(Nm[:], gps[:], [bgp], [bN])
                    Pm, bPm = sm_ring.next()
                    Pn, bPn = sm_ring.next()
                    dve(lambda e: e.tensor_tensor(out=Pm[:], in0=C["identF"][:], in1=Mm[:], op=ALU.subtract), [bM], [bPm])
                    dve(lambda e: e.tensor_tensor(out=Pn[:], in0=C["identF"][:], in1=Nm[:], op=ALU.subtract), [bN], [bPn])
                    for lvl in range(5):
                        last = lvl == 4
                        N2, bN2 = sm_ring.next()
                        yield
                        gps, bgp = gp_ring.next()
                        pe(lambda e: e.matmul(gps[:], lhsT=Mm[:], rhs=Nm[:], start=True, stop=True), [bM, bN], [bgp])
                        evac(N2[:], gps[:], [bgp], [bN2])
                        if not last:
                            M2, bM2 = sm_ring.next()
                            yield
                            gps, bgp = gp_ring.next()
                            pe(lambda e: e.matmul(gps[:], lhsT=Nm[:], rhs=Mm[:], start=True, stop=True), [bM, bN], [bgp])
                            evac(M2[:], gps[:], [bgp], [bM2])
                        Pn2, bPn2 = sm_ring.next()
                        yield
                        gps, bgp = gp_ring.next()
                        pe(lambda e: e.matmul(gps[:], lhsT=Pm[:], rhs=N2[:], start=True, stop=True), [bPm, bN2], [bgp])
                        dve(lambda e: e.tensor_tensor(out=Pn2[:], in0=gps[:], in1=Pn[:], op=ALU.add), [bgp, bPn], [bPn2])
                        if not last:
                            Pm2, bPm2 = sm_ring.next()
                            yield
                            gps, bgp = gp_ring.next()
                            pe(lambda e: e.matmul(gps[:], lhsT=Pn[:], rhs=M2[:], start=True, stop=True), [bPn, bM2], [bgp])
                            dve(lambda e: e.tensor_tensor(out=Pm2[:], in0=gps[:], in1=Pm[:], op=ALU.add),
                                [bgp, bPm], [bPm2])
                            Pm, bPm, Mm, bM = Pm2, bPm2, M2, bM2
                        Pn, bPn, Nm, bN = Pn2, bPn2, N2, bN2
                    TT, bTT = Pn, bPn
                    u_, wT_, qg_ = nb("u", par), nb("wT", par), nb("qg", par)
                    yield
                    gps, bgp = gp_ring.next()
                    pe(lambda e: e.matmul(gps[:], lhsT=TT[:], rhs=Vb[0][:], start=True, stop=True), [bTT, Vb[1]], [bgp])
                    evac(u_[0][:], gps[:], [bgp], [u_[1]])
                    yield
                    gps, bgp = gp_ring.next()
                    pe(lambda e: e.matmul(gps[:], lhsT=Kbg[0][:], rhs=TT[:], start=True, stop=True), [bTT, Kbg[1]], [bgp])
                    evac(wT_[0][:], gps[:], [bgp], [wT_[1]])
                    dg, bdg = sm_ring.next()
                    dve(lambda e: e.tensor_scalar(out=dg[:], in0=C["identF"][:], scalar1=egc_c, scalar2=None,
                                                  op0=ALU.mult), [b_st], [bdg])
                    yield
                    gps, bgp = gp_ring.next()
                    pe(lambda e: e.matmul(gps[:], lhsT=C["onesF"][:], rhs=dg[:], start=True, stop=True), [bdg], [bgp])
                    dve(lambda e: e.tensor_tensor(out=qg_[0][:], in0=gps[:], in1=QnT[:, tsl], op=ALU.mult),
                        [bgp, b_Qn], [qg_[1]])
                    o_sb = nb("o", par)
                    vn = nb("vn", par)
                    for j in range(2):
                        rs = slice(64 * j, 64 * j + 64)
                        S_, bS_ = Sst[scur], b_S[scur]
                        Sn, bSn = Sst[1 - scur], b_S[1 - scur]
                        yield
                        gps, bgp = gp_ring.next()
                        pe(lambda e: e.matmul(gps[:], lhsT=wT_[0][:], rhs=S_[:], start=True, stop=True),
                           [wT_[1], bS_], [bgp])
                        dve(lambda e: e.tensor_tensor(out=vn[0][rs, :], in0=u_[0][rs, :], in1=gps[rs, :], op=ALU.subtract),
                            [bgp, u_[1]], [vn[1]])
                        gpo, bgo = gp_ring.next()
                        pe(lambda e: e.matmul(gpo[:], lhsT=qg_[0][:], rhs=S_[:], start=True, stop=False),
                           [qg_[1], bS_], [bgo])
                        pe(lambda e: e.matmul(gpo[:], lhsT=Aq[0][rs, :], rhs=vn[0][rs, :], start=False, stop=True),
                           [Aq[1], vn[1]], [bgo])
                        yield
                        gps, bgp = gp_ring.next()
                        pe(lambda e: e.matmul(gps[:], lhsT=Ktl[0][rs, :], rhs=vn[0][rs, :], start=True, stop=True),
                           [Ktl[1], vn[1]], [bgp])
                        dve(lambda e: e.scalar_tensor_tensor(out=Sn[:], in0=S_[:], scalar=egl[0][:, j:j + 1], in1=gps[:],
                                                             op0=ALU.mult, op1=ALU.add), [bS_, egl[1], bgp], [bSn])
                        act(lambda e: e.activation(out=o_sb[0][rs, :], in_=gpo[rs, :], func=AF.Copy), [bgo], [o_sb[1]])
                        scur = 1 - scur
                    jk, bjk = sm_ring.next()
                    act(lambda e: e.activation(out=jk[:], in_=o_sb[0][:], func=AF.Square, accum_out=st[:, 26:27]),
                        [o_sb[1]], [bjk, b_st])
                    rstd_ln_exp(st[:, 26:27], 1.0 / 128, cst[:, 0:1])
                    dve(lambda e: e.scalar_tensor_tensor(out=jk[:], in0=o_sb[0][:], scalar=st[:, 26:27], in1=C["gnw"][:],
                                                         op0=ALU.mult, op1=ALU.mult), [o_sb[1], b_st, bjk], [bjk])
                    dve(lambda e: e.tensor_tensor(out=ybuf[t][:, 0:128], in0=jk[:], in1=zs[:, t, :], op=ALU.mult),
                        [bjk, b_zs[t]], [b_y[t]])

                return
                yield

            def moba_gen():
                act(lambda e: e.activation(out=sqQ[:], in_=QTf[:], func=AF.Square), [b_QTf], [b_sqQ])
                for t in (range(4) if stage >= 3 else []):
                    tsl = slice(t * 128, (t + 1) * 128)
                    blkq = b0 + t // 2
                    gps, bgp = acc_ring.next()
                    pe(lambda e: e.matmul(gps[:, 0:1], lhsT=sqQ[:, tsl], rhs=C["onesF"][:, 0:1], start=True, stop=True),
                       [b_sqQ], [bgp])
                    dve(lambda e: e.tensor_scalar(out=stm[:, 27:28], in0=gps[:, 0:1], scalar1=cst[:, 3:4], scalar2=1e-30,
                                                  op0=ALU.mult, op1=ALU.add), [bgp, b_cst], [b_stm])
                    act(lambda e: e.activation(out=stm[:, 27:28], in_=stm[:, 27:28], func=AF.Ln), [b_stm], [b_stm])
                    act(lambda e: e.activation(out=stm[:, 27:28], in_=stm[:, 27:28], func=AF.Exp, scale=0.5), [b_stm], [b_stm])
                    dve(lambda e: e.tensor_tensor(out=stm[:, 27:28], in0=stm[:, 27:28], in1=C["slopeT"][:, t:t + 1],
                                                  op=ALU.add), [b_stm], [b_stm])
                    dve(lambda e: e.tensor_scalar(out=stm[:, 27:28], in0=stm[:, 27:28], scalar1=-1.0, scalar2=NEG,
                                                  op0=ALU.mult, op1=ALU.add), [b_stm], [b_stm])
                    gm, bgm = smm_ring.next()
                    mk_, bmk = smm_ring.next()
                    gps, bgp = acc_ring.next()
                    pe(lambda e: e.matmul(gps[:, 0:NB], lhsT=QTf[:, tsl], rhs=kmT[:, 0:NB], start=True, stop=True),
                       [b_QTf, b_km], [bgp])
                    dve(lambda e: e.tensor_scalar(out=mk_[:, 0:NB], in0=C["jrow"][:, 0:NB], scalar1=float(blkq), scalar2=None,
                                                  op0=ALU.is_lt), [], [bmk])
                    dve(lambda e: e.tensor_tensor(out=gm[:, 0:NB], in0=gps[:, 0:NB], in1=mk_[:, 0:NB], op=ALU.mult),
                        [bgp, bmk], [bgm])
                    dve(lambda e: e.tensor_scalar(out=mk_[:, 64:64 + NB], in0=mk_[:, 0:NB], scalar1=1e30, scalar2=-1e30,
                                                  op0=ALU.mult, op1=ALU.add), [bmk], [bmk])
                    dve(lambda e: e.tensor_tensor(out=gm[:, 0:NB], in0=gm[:, 0:NB], in1=mk_[:, 64:64 + NB], op=ALU.add),
                        [bgm, bmk], [bgm])
                    dve(lambda e: e.max(out=stm[:, 0:8], in_=gm[:, 0:NB]), [bgm], [b_stm])
                    dve(lambda e: e.tensor_scalar(out=gm[:, 0:NB], in0=gm[:, 0:NB], scalar1=stm[:, 2:3], scalar2=None,
                                                  op0=ALU.is_ge), [bgm, b_stm], [bgm])
                    dve(lambda e: e.tensor_tensor(out=gm[:, 0:NB], in0=gm[:, 0:NB], in1=mk_[:, 0:NB], op=ALU.mult),
                        [bgm, bmk], [bgm])
                    dve(lambda e: e.tensor_scalar(out=mk_[:, 0:NB], in0=C["jrow"][:, 0:NB], scalar1=float(blkq), scalar2=None,
                                                  op0=ALU.is_equal), [bmk], [bmk])
                    dve(lambda e: e.tensor_tensor(out=gm[:, 0:NB], in0=gm[:, 0:NB], in1=mk_[:, 0:NB], op=ALU.add),
                        [bgm, bmk], [bgm])
                    dve(lambda e: e.tensor_scalar(out=gm[:, 64:64 + NB], in0=gm[:, 0:NB], scalar1=-NEG, scalar2=stm[:, 27:28],
                                                  op0=ALU.mult, op1=ALU.add), [bgm, b_stm], [bgm])
                    dve(lambda e: e.tensor_copy(out=xn[:, 0:NB], in_=gm[:, 64:64 + NB]), [bgm], [b_xn])
                    pe(lambda e: e.transpose(out=tpx[0:NB, 0, :], in_=xn[:, 0:NB], identity=C["identB"][:]),
                       [b_xn], [b_tpx])
                    evac(biasT[0:NB, tsl], tpx[0:NB, 0, :], [b_tpx], [b_bT])
                    yield
                nkt = gt0 + 4 if stage >= 3 else 0
                for kt in range(nkt):
                    ktl = kt - gt0
                    yield
                    jb = kt // 2
                    ps, bp = acc_ring.next()
                    pe(lambda e: e.matmul(ps[:], lhsT=KT_all[:, kt * 128:(kt + 1) * 128], rhs=QT[:], start=True, stop=False),
                       [b_KT, b_QT], [bp])
                    r0 = 32 * (jb // 32)
                    pe(lambda e: e.matmul(ps[:], lhsT=C["sel32"][r0:r0 + KBK, (jb % 32) * 128:(jb % 32 + 1) * 128],
                                          rhs=biasT[r0:r0 + KBK, :], start=False, stop=(ktl < 0)), [b_bT], [bp])
                    if ktl >= 0:
                        pe(lambda e: e.matmul(ps[:], lhsT=C["identB"][:], rhs=C["cmask"][:, ktl * 512:(ktl + 1) * 512],
                                              start=False, stop=True), [], [bp])
                    pt, bpt = pt_ring.next()
                    ai = 3 - ktl
                    act(lambda e: e.activation(out=pt[:], in_=ps[:], func=AF.Exp, bias=C["alibiT"][:, ai:ai + 1]),
                        [bp], [bpt])
                    for qt in range(4):
                        if ktl > qt:
                            continue
                        last_kt = gt0 + qt
                        pe(lambda e: e.matmul(o_ps(qt), lhsT=pt[:, qt * 128:(qt + 1) * 128], rhs=V_all[:, kt, 0:129],
                                              start=(kt == 0 and qt % 2 == 0), stop=(kt == last_kt),
                                              skip_group_check=True), [bpt, b_V], [b_ops[qt]])
                return
                yield
            gens = [[gdn_gen() if stage >= 2 else iter(()), 0, 150.0], [moba_gen(), 0, float(gt0 + 9)]]
            alive = [True, True]
            while alive[0] or alive[1]:
                if alive[0] and alive[1]:
                    i = 0 if gens[0][1] / gens[0][2] <= gens[1][1] / gens[1][2] else 1
                else:
                    i = 0 if alive[0] else 1
                try:
                    next(gens[i][0])
                    gens[i][1] += 1
                except StopIteration:
                    alive[i] = False
            for qt in range(4):
                if stage >= 3:
                    dve(lambda e: e.reciprocal(out=stm[:, 28:29], in_=o_ps(qt)[:, 128:129]), [b_ops[qt]], [b_stm])
                if stage >= 3:
                    dve(lambda e: e.tensor_scalar(out=ybuf[qt][:, 128:256], in0=o_ps(qt)[:, 0:128], scalar1=stm[:, 28:29],
                                              scalar2=None, op0=ALU.mult), [b_ops[qt], b_stm], [b_y[qt]])
                out_toks.append(k.dma("sp", d_y[qt], y_d[n0 + qt * 128:n0 + (qt + 1) * 128, :], ybuf[qt][:],
                                      reads=[b_y[qt]], force=True))
    k.finish(out_toks)
    print("build_a nops", k.nops, flush=True)
    k.close()
    return nc


def inputs_a(T, core, x2d, w_in, conv_w, a_log, dt_bias, gdn_norm_w, pre_mix_norm):
    c = core
    cols = np.concatenate([np.arange(c * 128, c * 128 + 128), 1024 + np.arange(c * 128, c * 128 + 128),
                           2048 + np.arange(c * 128, c * 128 + 128), 4112 + np.arange(c * 128, c * 128 + 128),
                           5136 + np.arange(c * 128, c * 128 + 128), 3072 + np.arange(c * 128, c * 128 + 128),
                           6160 + np.arange(c * 128, c * 128 + 128), [4096 + c], [4104 + c]]).astype(np.int64)
    d = consts_a(T, c)
    d["x"] = x2d
    d["w_in"] = np.ascontiguousarray(w_in[:, cols])
    cw = np.zeros((128, 16), np.float32)
    for ci in range(3):
        for j in range(4):
            cw[:, ci * 4 + j] = conv_w[j, ci * 1024 + c * 128:ci * 1024 + (c + 1) * 128]
    d["convw"] = cw
    d["alog"] = np.full((128, 16), a_log[c], np.float32)
    d["dtb"] = np.full((128, 16), dt_bias[c], np.float32)
    d["gnw"] = _rep(gdn_norm_w)
    d["gpre"] = np.ascontiguousarray(np.asarray(pre_mix_norm, np.float32).reshape(NKC, 128).T)
    return d


_NC_CACHE = {}


def kernel(x, mem, pre_mix_norm, w_in, conv_w, a_log, dt_bias, gdn_norm_w, w_out, post_mix_norm,
           pre_mem_norm, mem_kv_norm, w_mq, w_mk, w_mv, w_mo, post_mem_norm,
           pre_mlp_norm, w_up, w_down, post_mlp_norm):
    f32 = lambda a: np.ascontiguousarray(np.asarray(a, dtype=np.float32))
    x = f32(x)
    B, T, _ = x.shape
    N = B * T
    x2d = x.reshape(N, D)
    if ("a", T) not in _NC_CACHE:
        _NC_CACHE[("a", T)] = build_a(T)
    ins_a = [inputs_a(T, c, x2d, f32(w_in)[0], f32(conv_w)[0], f32(a_log)[0], f32(dt_bias)[0], f32(gdn_norm_w)[0],
                      f32(pre_mix_norm)[0]) for c in range(8)]
    res_a = run_bass_kernel_spmd(_NC_CACHE[("a", T)], ins_a, core_ids=list(range(8)))
    y = np.empty((N, D), dtype=ml_dtypes.bfloat16)
    for c in range(8):
        yc = np.asarray(res_a.results[c]["y"])
        y[:, c * 128:(c + 1) * 128] = yc[:, 0:128]
        y[:, 1024 + c * 128:1024 + (c + 1) * 128] = yc[:, 128:256]
    NTOK = N // 8
    if ("b", NTOK) not in _NC_CACHE:
        _NC_CACHE[("b", NTOK)] = build_b(NTOK)
    ident = np.eye(128, dtype=ml_dtypes.bfloat16)
    shared = {"w_out": f32(w_out)[0], "w_mq": f32(w_mq)[0], "w_mk": f32(w_mk)[0], "w_mv": f32(w_mv)[0],
              "w_mo": f32(w_mo)[0], "w_up": f32(w_up)[0], "w_down": f32(w_down)[0], "ident": ident,
              "post_mix_norm": _rep(f32(post_mix_norm)[0]), "pre_mem_norm": _rep(f32(pre_mem_norm)[0]),
              "mem_kv_norm": _rep(f32(mem_kv_norm)[0]), "post_mem_norm": _rep(f32(post_mem_norm)[0]),
              "pre_mlp_norm": _rep(f32(pre_mlp_norm)[0]), "post_mlp_norm": _rep(f32(post_mlp_norm)[0])}
    memf = f32(mem)
    ins_b = []
    for c in range(8):
        d = dict(shared)
        d["x"] = np.ascontiguousarray(x2d[c * NTOK:(c + 1) * NTOK])
        d["y"] = np.ascontiguousarray(y[c * NTOK:(c + 1) * NTOK])
        d["mem"] = np.ascontiguousarray(memf[(c * NTOK) // T])
        ins_b.append(d)
    res_b = run_bass_kernel_spmd(_NC_CACHE[("b", NTOK)], ins_b, core_ids=list(range(8)))
    out = np.concatenate([np.asarray(r["out"]) for r in res_b.results], 0)
    return out.reshape(B, T, D).astype(np.float32)
```
